# Optimizing a Trainium2 kernel written in Bass

```python
import math
import jax, jax.numpy as jnp
from jax import lax
import numpy as np

D_MODEL = 1024
BATCH = 4
SEQ = 4096
DEPTH = 1

N_ATTN_HEADS = 8
SUB_HEAD_DIM = 64
V_HEAD_DIM = 2 * SUB_HEAD_DIM
ATTN_WIDTH = N_ATTN_HEADS * V_HEAD_DIM
ROPE_THETA = 10000.0
Q_BLOCK = 128
LAMBDA_BASE = 0.8
LAMBDA_AMP = 0.6
LAMBDA_RATE = 0.3

LRU_WIDTH = 1024
LRU_BLOCKS = 8
LRU_BLOCK_DIM = LRU_WIDTH // LRU_BLOCKS
CONV_WIDTH = 4
CONV_PAD_LEFT = (CONV_WIDTH - 1) // 2
CONV_PAD_RIGHT = CONV_WIDTH - 1 - CONV_PAD_LEFT
LRU_C = 8.0
N_DIRECTIONS = 2

N_GATED_BRANCHES = 2
PROJ_WIDTH = 3 * ATTN_WIDTH + 2 * LRU_WIDTH + N_GATED_BRANCHES * D_MODEL
SPLIT_POINTS = (ATTN_WIDTH, 2 * ATTN_WIDTH, 3 * ATTN_WIDTH,
                3 * ATTN_WIDTH + LRU_WIDTH, 3 * ATTN_WIDTH + 2 * LRU_WIDTH)

N_GROUPS = 4
EXPERTS_PER_GROUP = 4
N_EXPERTS = N_GROUPS * EXPERTS_PER_GROUP
TOP_K_IN_GROUP = 2
EXPERT_HIDDEN = 512
TOKEN_BLOCK = 128

RMS_EPS = 1e-6

kernel_name = "hybrid_diffattn_rglru_hmoe_encoder"


def rms_norm(x, gain):
    xf = x.astype(jnp.float32)
    y = xf * lax.rsqrt(jnp.mean(xf * xf, axis=-1, keepdims=True) + RMS_EPS)
    return (y * gain.astype(jnp.float32)).astype(x.dtype)


def lambda_init_fn(layer_idx):
    return LAMBDA_BASE - LAMBDA_AMP * math.exp(-LAMBDA_RATE * layer_idx)


def rope_tables(seq, dim):
    pos = jnp.arange(seq, dtype=jnp.float32)
    inv_freq = ROPE_THETA ** (-jnp.arange(0, dim, 2, dtype=jnp.float32) / dim)
    ang = pos[:, None] * inv_freq[None, :]
    return jnp.cos(ang), jnp.sin(ang)


def apply_rope(x, cos, sin):
    xf = x.astype(jnp.float32)
    x1, x2 = jnp.split(xf, 2, axis=-1)
    c = cos[None, :, None, None, :]
    s = sin[None, :, None, None, :]
    return jnp.concatenate([x1 * c - x2 * s, x1 * s + x2 * c], axis=-1).astype(x.dtype)


def diff_attention(q, k, v, lam):
    B, S, H, _, dh = q.shape
    scale = dh ** -0.5
    n_blk = S // Q_BLOCK
    qb = q.reshape(B, n_blk, Q_BLOCK, H, 2, dh).transpose(1, 0, 2, 3, 4, 5)

    def one_block(q_blk):
        s = jnp.einsum('bqhcd,bkhcd->bhcqk', q_blk, k,
                       preferred_element_type=jnp.float32) * scale
        p = jax.nn.softmax(s, axis=-1)
        p_diff = p[:, :, 0] - lam * p[:, :, 1]
        return jnp.einsum('bhqk,bkhd->bqhd', p_diff.astype(v.dtype), v)

    out = lax.map(one_block, qb)
    return out.transpose(1, 0, 2, 3, 4).reshape(B, S, H, v.shape[-1])


def centred_depthwise_conv(x, w, b):
    y = lax.conv_general_dilated(
        x, w[:, None, :], window_strides=(1,),
        padding=[(CONV_PAD_LEFT, CONV_PAD_RIGHT)],
        dimension_numbers=('NWC', 'WIO', 'NWC'),
        feature_group_count=x.shape[-1])
    return y + b


def block_diag_linear(x, w, b):
    B, S, C = x.shape
    xb = x.reshape(B, S, LRU_BLOCKS, LRU_BLOCK_DIM)
    y = jnp.einsum('bsnd,nde->bsne', xb, w)
    return y.reshape(B, S, C) + b


def rg_lru(x, w_a, b_a, w_i, b_i, lam_param, reverse):
    r = jax.nn.sigmoid(block_diag_linear(x, w_a, b_a).astype(jnp.float32))
    i = jax.nn.sigmoid(block_diag_linear(x, w_i, b_i).astype(jnp.float32))
    log_a = -LRU_C * r * jax.nn.softplus(-lam_param.astype(jnp.float32))
    a = jnp.exp(log_a)
    mult = jnp.sqrt(-jnp.expm1(2.0 * log_a))
    u = mult * (i * x.astype(jnp.float32))

    def combine(c1, c2):
        a1, b1 = c1
        a2, b2 = c2
        return a1 * a2, a2 * b1 + b2

    _, h = lax.associative_scan(combine, (a, u), axis=1, reverse=reverse)
    return h.astype(x.dtype)


def hierarchical_moe(h, w_group, b_group, w_er, b_er, w_gate, w_up, w_down):
    B, S, D = h.shape
    N = B * S
    xt = h.reshape(N, D)
    g_logits = (xt @ w_group + b_group).astype(jnp.float32)
    g_prob = jax.nn.softmax(g_logits, axis=-1)
    g_sel = jnp.argmax(g_logits, axis=-1)
    g_w = jnp.take_along_axis(g_prob, g_sel[:, None], axis=-1)
    e_logits = (xt @ w_er + b_er).astype(jnp.float32).reshape(N, N_GROUPS, EXPERTS_PER_GROUP)
    e_sel_logits = jnp.take_along_axis(e_logits, g_sel[:, None, None], axis=1)[:, 0]
    top_val, top_idx = lax.top_k(e_sel_logits, TOP_K_IN_GROUP)
    top_w = jax.nn.softmax(top_val, axis=-1) * g_w
    expert_id = g_sel[:, None] * EXPERTS_PER_GROUP + top_idx
    combine_w = jnp.sum(jax.nn.one_hot(expert_id, N_EXPERTS, dtype=jnp.float32)
                        * top_w[..., None], axis=1)

    n_blk = S // TOKEN_BLOCK
    xb = h.reshape(B, n_blk, TOKEN_BLOCK, D).transpose(1, 0, 2, 3).reshape(n_blk, B * TOKEN_BLOCK, D)
    cb = combine_w.reshape(B, n_blk, TOKEN_BLOCK, N_EXPERTS).transpose(1, 0, 2, 3)
    cb = cb.reshape(n_blk, B * TOKEN_BLOCK, N_EXPERTS).astype(h.dtype)

    def expert_block(args):
        xs, cs = args
        a = jnp.einsum('nd,edf->nef', xs, w_gate)
        u = jnp.einsum('nd,edf->nef', xs, w_up)
        hid = jax.nn.silu(a) * u * cs[:, :, None]
        return jnp.einsum('nef,efd->nd', hid, w_down)

    y = lax.map(expert_block, (xb, cb))
    y = y.reshape(n_blk, B, TOKEN_BLOCK, D).transpose(1, 0, 2, 3)
    return y.reshape(B, S, D)


def setup_inputs(seed: int = 0) -> dict:
    key = jax.random.key(seed)
    ks = jax.random.split(key, 32)
    f32 = jnp.float32
    nrm = lambda k, shape, scale: jax.random.normal(k, shape, f32) * scale
    gain = lambda k, shape: jnp.ones(shape, f32) + 0.02 * jax.random.normal(k, shape, f32)

    a_c = jax.random.uniform(ks[16], (DEPTH, N_DIRECTIONS, LRU_WIDTH), f32, 0.9, 0.999)
    a_base = a_c ** (1.0 / LRU_C)
    lru_lambda = jnp.log(a_base) - jnp.log1p(-a_base)

    return {
        "x": jax.random.normal(ks[0], (BATCH, SEQ, D_MODEL), f32),
        "norm1_gain": gain(ks[1], (DEPTH, D_MODEL)),
        "w_in": nrm(ks[2], (DEPTH, D_MODEL, PROJ_WIDTH), D_MODEL ** -0.5),
        "b_gates": nrm(ks[3], (DEPTH, N_GATED_BRANCHES * D_MODEL), 0.02),
        "q_norm_gain": gain(ks[4], (DEPTH, SUB_HEAD_DIM)),
        "k_norm_gain": gain(ks[5], (DEPTH, SUB_HEAD_DIM)),
        "lambda_q1": nrm(ks[6], (DEPTH, SUB_HEAD_DIM), 0.1),
        "lambda_k1": nrm(ks[7], (DEPTH, SUB_HEAD_DIM), 0.1),
        "lambda_q2": nrm(ks[8], (DEPTH, SUB_HEAD_DIM), 0.1),
        "lambda_k2": nrm(ks[9], (DEPTH, SUB_HEAD_DIM), 0.1),
        "attn_subln_gain": gain(ks[10], (DEPTH, V_HEAD_DIM)),
        "w_attn_o": nrm(ks[11], (DEPTH, ATTN_WIDTH, D_MODEL), ATTN_WIDTH ** -0.5),
        "conv_w": nrm(ks[12], (DEPTH, CONV_WIDTH, LRU_WIDTH), CONV_WIDTH ** -0.5),
        "conv_b": nrm(ks[13], (DEPTH, LRU_WIDTH), 0.02),
        "lru_wa": nrm(ks[14], (DEPTH, N_DIRECTIONS, LRU_BLOCKS, LRU_BLOCK_DIM, LRU_BLOCK_DIM), LRU_BLOCK_DIM ** -0.5),
        "lru_ba": nrm(ks[15], (DEPTH, N_DIRECTIONS, LRU_WIDTH), 0.02),
        "lru_wi": nrm(ks[17], (DEPTH, N_DIRECTIONS, LRU_BLOCKS, LRU_BLOCK_DIM, LRU_BLOCK_DIM), LRU_BLOCK_DIM ** -0.5),
        "lru_bi": nrm(ks[18], (DEPTH, N_DIRECTIONS, LRU_WIDTH), 0.02),
        "lru_lambda": lru_lambda,
        "w_lru_o": nrm(ks[19], (DEPTH, LRU_WIDTH, D_MODEL), LRU_WIDTH ** -0.5),
        "w_out": nrm(ks[20], (DEPTH, D_MODEL, D_MODEL), D_MODEL ** -0.5),
        "norm2_gain": gain(ks[21], (DEPTH, D_MODEL)),
        "w_group_router": nrm(ks[22], (DEPTH, D_MODEL, N_GROUPS), D_MODEL ** -0.5),
        "b_group_router": nrm(ks[23], (DEPTH, N_GROUPS), 0.01),
        "w_expert_router": nrm(ks[24], (DEPTH, D_MODEL, N_EXPERTS), D_MODEL ** -0.5),
        "b_expert_router": nrm(ks[25], (DEPTH, N_EXPERTS), 0.01),
        "w_expert_gate": nrm(ks[26], (DEPTH, N_EXPERTS, D_MODEL, EXPERT_HIDDEN), D_MODEL ** -0.5),
        "w_expert_up": nrm(ks[27], (DEPTH, N_EXPERTS, D_MODEL, EXPERT_HIDDEN), D_MODEL ** -0.5),
        "w_expert_down": nrm(ks[28], (DEPTH, N_EXPERTS, EXPERT_HIDDEN, D_MODEL), EXPERT_HIDDEN ** -0.5),
    }


def reference(x, norm1_gain, w_in, b_gates, q_norm_gain, k_norm_gain,
              lambda_q1, lambda_k1, lambda_q2, lambda_k2, attn_subln_gain, w_attn_o,
              conv_w, conv_b, lru_wa, lru_ba, lru_wi, lru_bi, lru_lambda, w_lru_o,
              w_out, norm2_gain, w_group_router, b_group_router,
              w_expert_router, b_expert_router, w_expert_gate, w_expert_up, w_expert_down):
    B, S, D = x.shape
    cos, sin = rope_tables(S, SUB_HEAD_DIM)
    for l in range(DEPTH):
        lam_init = lambda_init_fn(l)
        h = rms_norm(x, norm1_gain[l])
        proj = jnp.einsum('bsd,dc->bsc', h, w_in[l])
        q, k, v, lru_x, lru_g, gate_pre = jnp.split(proj, SPLIT_POINTS, axis=-1)

        q = q.reshape(B, S, N_ATTN_HEADS, 2, SUB_HEAD_DIM)
        k = k.reshape(B, S, N_ATTN_HEADS, 2, SUB_HEAD_DIM)
        v = v.reshape(B, S, N_ATTN_HEADS, V_HEAD_DIM)
        q = apply_rope(rms_norm(q, q_norm_gain[l]), cos, sin)
        k = apply_rope(rms_norm(k, k_norm_gain[l]), cos, sin)
        lam = (jnp.exp(jnp.sum(lambda_q1[l].astype(jnp.float32) * lambda_k1[l].astype(jnp.float32)))
               - jnp.exp(jnp.sum(lambda_q2[l].astype(jnp.float32) * lambda_k2[l].astype(jnp.float32)))
               + lam_init)
        o = diff_attention(q, k, v, lam)
        o = rms_norm(o, attn_subln_gain[l]) * (1.0 - lam_init)
        attn_d = jnp.einsum('bsc,cd->bsd', o.reshape(B, S, ATTN_WIDTH), w_attn_o[l])

        xr = centred_depthwise_conv(lru_x, conv_w[l], conv_b[l])
        h_fwd = rg_lru(xr, lru_wa[l, 0], lru_ba[l, 0], lru_wi[l, 0], lru_bi[l, 0], lru_lambda[l, 0], False)
        h_bwd = rg_lru(xr, lru_wa[l, 1], lru_ba[l, 1], lru_wi[l, 1], lru_bi[l, 1], lru_lambda[l, 1], True)
        y_lru = (h_fwd + h_bwd) * jax.nn.gelu(lru_g)
        lru_d = jnp.einsum('bsc,cd->bsd', y_lru, w_lru_o[l])

        g_attn, g_lru = jnp.split(jax.nn.sigmoid(gate_pre + b_gates[l]), N_GATED_BRANCHES, axis=-1)
        merged = g_attn * attn_d + g_lru * lru_d
        x = x + jnp.einsum('bsd,de->bse', merged, w_out[l])

        h2 = rms_norm(x, norm2_gain[l])
        x = x + hierarchical_moe(h2, w_group_router[l], b_group_router[l],
                                 w_expert_router[l], b_expert_router[l],
                                 w_expert_gate[l], w_expert_up[l], w_expert_down[l])
    return x
```

```python
import math
import numpy as np
from contextlib import ExitStack
import concourse.bass as bass
import concourse.mybir as mybir
from concourse.bass_utils import run_bass_kernel_spmd


F32 = mybir.dt.float32
BF16 = mybir.dt.bfloat16
AF = mybir.ActivationFunctionType
ALU = mybir.AluOpType
AX = mybir.AxisListType

EPOCH = 16000
NDMASEM = 8


class Op:
    __slots__ = ("eng", "fn", "deps", "sig", "sigcount", "dma", "dsem", "dval", "dprev", "gi")


class Prog:
    ENG = ["pe", "act", "dve", "pool", "sp"]

    def __init__(self):
        self.q = {e: [] for e in self.ENG}
        self.last_w = {}
        self.readers = {}
        self.ndma = {e: 0 for e in self.ENG}
        self.n = 0

    def op(self, eng, name, reads=(), writes=(), **kw):
        return self.add(eng, (name, kw), reads, writes, False)

    def dma(self, eng, reads=(), writes=(), **kw):
        return self.add(eng, ("dma_start", kw), reads, writes, True)

    def barrier(self):
        lastc = {}
        lastd = []
        for e in self.ENG:
            cs = [o for o in self.q[e] if not o.dma]
            if cs:
                lastc[e] = cs[-1]
            ds = [o for o in self.q[e] if o.dma]
            lastd += ds[-NDMASEM:]
        for e in self.ENG:
            op = self.add(e, ("nop", {}), (), (), False)
            op.deps = [o for pe, o in lastc.items() if pe != e] + list(lastd)
            for d in op.deps:
                if not d.dma:
                    d.sig = True

    def add(self, eng, fn, reads=(), writes=(), dma=False):
        op = Op()
        op.eng = eng
        op.fn = fn
        op.dma = dma
        op.sig = False
        op.sigcount = 0
        op.gi = self.n
        self.n += 1
        raw = set()
        deps = set()
        for s in reads:
            w = self.last_w.get(s)
            if w is not None:
                raw.add(w)
        for s in writes:
            w = self.last_w.get(s)
            if w is not None:
                deps.add(w)
            for r in self.readers.get(s, ()):
                deps.add(r)
        out = []
        for d in raw | deps:
            if d is op:
                continue
            if d.eng == eng and not d.dma and not dma and d not in raw and eng == "pe":
                continue
            out.append(d)
        op.deps = out
        for d in out:
            if not d.dma:
                d.sig = True
        if dma:
            n = self.ndma[eng]
            self.ndma[eng] = n + 1
            op.dsem = n % NDMASEM
            op.dval = 16 * (n // NDMASEM + 1)
            op.dprev = 16 * (n // NDMASEM)
        for s in reads:
            self.readers.setdefault(s, []).append(op)
        for s in writes:
            self.last_w[s] = op
            self.readers[s] = []
        self.q[eng].append(op)
        return op

    def emit(self, nc, stack, final_wait_ops=()):
        nsem = {}
        for e in self.ENG:
            c = 0
            for op in self.q[e]:
                if op.sig and not op.dma:
                    c += 1
                    op.sigcount = c
            nsem[e] = (c + EPOCH - 1) // EPOCH
        csem = {e: [stack.enter_context(nc.semaphore(f"c_{e}_{i}")) for i in range(nsem[e])] for e in self.ENG}
        dsem = {e: [stack.enter_context(nc.semaphore(f"d_{e}_{i}")) for i in range(NDMASEM)] if self.ndma[e] else [] for e in self.ENG}
        block = stack.enter_context(nc.Block())
        deco = {"pe": block.tensor, "act": block.scalar, "dve": block.vector, "pool": block.gpsimd, "sp": block.sync}
        stats = {}

        def make(e):
            def body(eng):
                waited_c = {p: 0 for p in self.ENG}
                waited_d = {}
                nw = 0
                for op in self.q[e]:
                    need_c = {}
                    need_d = {}
                    for d in op.deps:
                        if d.dma:
                            k = (d.eng, d.dsem)
                            if waited_d.get(k, 0) < d.dval:
                                need_d[k] = max(need_d.get(k, 0), d.dval)
                        else:
                            if waited_c[d.eng] < d.sigcount:
                                need_c[d.eng] = max(need_c.get(d.eng, 0), d.sigcount)
                    if op.dma and op.dprev > 0:
                        k = (e, op.dsem)
                        if waited_d.get(k, 0) < op.dprev:
                            need_d[k] = max(need_d.get(k, 0), op.dprev)
                    for p, cnt in need_c.items():
                        si = (cnt - 1) // EPOCH
                        eng.wait_ge(csem[p][si], (cnt - 1) % EPOCH + 1)
                        waited_c[p] = cnt
                        nw += 1
                    for k, v in need_d.items():
                        eng.wait_ge(dsem[k[0]][k[1]], v)
                        waited_d[k] = v
                        nw += 1
                    ins = getattr(eng, op.fn[0])(**op.fn[1])
                    if op.dma:
                        ins.then_inc(dsem[e][op.dsem], 16)
                    elif op.sig:
                        si = (op.sigcount - 1) // EPOCH
                        ins.then_inc(csem[e][si], 1)
                if e == "sp":
                    for d in final_wait_ops:
                        if d.dma:
                            eng.wait_ge(dsem[d.eng][d.dsem], d.dval)
                        else:
                            si = (d.sigcount - 1) // EPOCH
                            eng.wait_ge(csem[d.eng][si], (d.sigcount - 1) % EPOCH + 1)
                stats[e] = (len(self.q[e]), nw)
            return body

        for e in self.ENG:
            deco[e](make(e))
        return stats


D = 1024
LAM_INIT = 0.8 - 0.6 * math.exp(-0.3 * 0)


class Cfg:
    def __init__(self, T=4096, NH=8, stages="ABC", debug=(), NB=8, NE=16):
        self.T = T
        self.TO = T // 2
        self.NT = T // 128
        self.NTO = self.TO // 128
        self.NCH = T // 512
        self.NCHO = self.TO // 512
        self.NH = NH
        self.stages = stages
        self.NB = NB
        self.NE = NE
        self.debug = debug


def build(cfg):
    T, TO, NT, NTO, NCH, NCHO, NH = cfg.T, cfg.TO, cfg.NT, cfg.NTO, cfg.NCH, cfg.NCHO, cfg.NH
    nc = bass.Bass("TRN2", target_bir_lowering=False)
    din = lambda name, shape, dt=F32: nc.dram_tensor(name, shape, dt, kind="ExternalInput").ap()
    dout = lambda name, shape, dt=F32: nc.dram_tensor(name, shape, dt, kind="ExternalOutput").ap()
    x_d = din("x", [T, D])
    g1_d = din("g1", [1, D])
    cs_d = din("cs", [T, 64])
    gq_d = din("gq", [1, 64])
    gk_d = din("gk", [1, 64])
    lam_d = din("lamv", [4, 64])
    gsub_d = din("gsub", [1, 128])
    wqkv_d = din("wqkv", [NH, 128, 8, 384])
    cp_d = din("cp", [128, 8, 12])
    wlx_d = din("wlx", [8, 128, 8, 128])
    wlg_d = din("wlg", [8, 128, 8, 128])
    wg_d = din("wg", [8, 128, 4, 128])
    NB = cfg.NB
    NE = cfg.NE
    wao_d = din("wao", [8, 128, 8, 128])
    wlo_d = din("wlo", [8, 128, 8, 128])
    wga_d = din("wga", [8, 128, 8, 128])
    wgl_d = din("wgl", [8, 128, 8, 128])
    bg_d = din("bg", [128, 16])
    wout_d = din("wout", [128, 8, 1024])
    g2_d = din("g2", [1, D])
    wr_d = din("wr", [128, 8, 20])
    br_d = din("br", [1, 20])
    weg_d = din("weg", [16, 128, 8, 512])
    weu_d = din("weu", [16, 128, 8, 512])
    wed_d = din("wed", [16, 128, 4, 1024])
    out_d = dout("out", [TO, D])
    dbg = {}
    if "oT" in cfg.debug:
        dbg["oT"] = dout("dbg_oT", [128, NH, TO], BF16)
    if "yT" in cfg.debug:
        dbg["yT"] = dout("dbg_yT", [128, 8, TO], BF16)
    if "KT" in cfg.debug:
        dbg["KT"] = dout("dbg_KT", [128, T], BF16)
        dbg["QT"] = dout("dbg_QT", [128, TO], BF16)
        dbg["V"] = dout("dbg_V", [128, NT, 130], BF16)

    P = Prog()
    fin = []
    with ExitStack() as st:
        sb = lambda name, shape, dt: st.enter_context(nc.sbuf_tensor(name, shape, dt))
        psum = lambda name, shape, dt: st.enter_context(nc.psum_tensor(name, shape, dt))
        hT = sb("hT", [128, 8, T], BF16)
        oT = sb("oT", [128, NH, TO], BF16)

        ident = sb("ident", [128, 128], BF16)
        identf = sb("identf", [128, 128], F32)
        eps = sb("eps", [128, 1], F32)
        neghalf = sb("neghalf", [128, 32], F32)
        gq = sb("gq_s", [128, 64], F32)
        gk = sb("gk_s", [128, 64], F32)
        lamv = sb("lamv_s", [128, 4, 64], F32)
        gsub = sb("gsub_s", [128, 128], F32)
        lam_t = sb("lam_t", [128, 8], F32)
        lam_j = sb("lam_j", [128, 2, 64], F32)
        cp = sb("cp_s", [128, 8, 12], F32)
        hbias = sb("hbias", [128, 8, 4], F32)
        hc = sb("hc", [128, 8, 2], F32)
        lrt = sb("lrt", [128, 8, 2], F32)
        sT = ExitStack()
        TK = sT.enter_context(nc.sbuf_tensor("TK", [128, NT, 4, 32], F32))
        TQ = sT.enter_context(nc.sbuf_tensor("TQ", [128, NTO, 4, 32], F32))
        sA = ExitStack()
        sbA = lambda name, shape, dt: sA.enter_context(nc.sbuf_tensor(name, shape, dt))
        gain1 = sbA("gain1", [128, D], F32)
        cs = sbA("cs_s", [128, NT, 64], F32)

        P.op("pool", "memset", writes=["identf"], ap=identf[:], constant=0.0)
        P.op("pool", "affine_select", reads=["identf"], writes=["identf"], out=identf[:], in_=identf[:], pattern=[[-1, 128]],
                                                 compare_op=ALU.not_equal, fill=1.0, base=0, channel_multiplier=1)
        P.op("pool", "tensor_copy", reads=["identf"], writes=["ident"], out=ident[:], in_=identf[:])
        P.op("pool", "memset", writes=["eps"], ap=eps[:], constant=1e-6)
        P.op("pool", "memset", writes=["neghalf"], ap=neghalf[:], constant=-0.5)
        P.dma("sp", writes=["gain1"], out=gain1[:], in_=g1_d.partition_broadcast(128))
        P.dma("sp", writes=["gq"], out=gq[:], in_=gq_d.partition_broadcast(128))
        P.dma("sp", writes=["gk"], out=gk[:], in_=gk_d.partition_broadcast(128))
        P.dma("sp", writes=["gsub"], out=gsub[:], in_=gsub_d.partition_broadcast(128))
        for i in range(4):
            P.dma("sp", writes=[("lamv", i)], out=lamv[:, i, :], in_=lam_d[i:i + 1, :].partition_broadcast(128))
        P.dma("sp", writes=["cs"], out=cs[:], in_=cs_d.rearrange("(n p) f -> p n f", p=128))
        for (Tt, g, gname, n, tname) in ((TK, gk, "gk", NT, "TK"), (TQ, gq, "gq", NTO, "TQ")):
            for ti, (csoff, goff) in enumerate(((0, 0), (32, 32), (32, 0), (0, 32))):
                P.op("pool", "tensor_tensor", reads=["cs", gname], writes=[tname],
                    out=Tt[:, :, ti, :], in0=cs[:, 0:n, csoff:csoff + 32],
                    in1=g[:, goff:goff + 32].unsqueeze(1).to_broadcast([128, n, 32]), op=ALU.mult)
        for i in range(2):
            P.op("dve", "tensor_tensor", reads=[("lamv", 2 * i), ("lamv", 2 * i + 1)], writes=[("lam_j", i)], out=lam_j[:, i, :], in0=lamv[:, 2 * i, :], in1=lamv[:, 2 * i + 1, :], op=ALU.mult)
        P.op("dve", "tensor_reduce", reads=[("lam_j", 0), ("lam_j", 1)], writes=["lam_t01"], out=lam_t[:, 0:2], in_=lam_j[:], axis=AX.X, op=ALU.add)
        P.op("act", "activation", reads=["lam_t01"], writes=["lam_t23"], out=lam_t[:, 2:4], in_=lam_t[:, 0:2], func=AF.Exp)
        P.op("dve", "tensor_tensor", reads=["lam_t23"], writes=["lam_t4"], out=lam_t[:, 4:5], in0=lam_t[:, 2:3], in1=lam_t[:, 3:4], op=ALU.subtract)
        P.op("dve", "tensor_scalar", reads=["lam_t4"], writes=["neglam"], out=lam_t[:, 5:6], in0=lam_t[:, 4:5], scalar1=-1.0, scalar2=-LAM_INIT,
                                               op0=ALU.mult, op1=ALU.add)
        P.op("dve", "tensor_scalar", reads=["gsub"], writes=["gsub"], out=gsub[:], in0=gsub[:], scalar1=(1.0 - LAM_INIT), scalar2=None, op0=ALU.mult)

        xt = sbA("xt", [128, 4, D], F32)
        junk = sbA("junk", [128, D], BF16)
        ssA = sbA("ssA", [128, 2, 4], F32)
        msA = sbA("msA", [128, 2, 4], F32)
        rsA = sbA("rsA", [128, 2, 4], F32)
        hb = sbA("hb", [128, 2, 4, D], BF16)
        banks = [psum(f"bank{i}", [128, 512], F32) for i in range(8)]
        bk_bf = [b[:].bitcast(BF16) for b in banks]

        nb = 0
        for c in range(NCH):
            cb = c % 2
            for j in range(4):
                tt = c * 4 + j
                P.dma("sp", writes=[("xt", j)], out=xt[:, j, :], in_=x_d[tt * 128:(tt + 1) * 128, :])
                P.op("act", "activation", reads=[("xt", j)], writes=["junk", ("ssA", cb, j)], out=junk[:], in_=xt[:, j, :], func=AF.Square,
                                                               accum_out=ssA[:, cb, j:j + 1])
            P.op("dve", "tensor_scalar", reads=[("ssA", cb, j) for j in range(4)], writes=[("msA", cb)], out=msA[:, cb, :], in0=ssA[:, cb, :], scalar1=1.0 / D, scalar2=1e-6,
                                                         op0=ALU.mult, op1=ALU.add)
            P.op("pool", "tensor_tensor", reads=[("msA", cb), "neghalf"], writes=[("rsA", cb)], out=rsA[:, cb, :], in0=msA[:, cb, :], in1=neghalf[:, 0:4], op=ALU.pow)
            for j in range(4):
                P.op("dve", "scalar_tensor_tensor", reads=[("xt", j), ("rsA", cb), "gain1"], writes=[("hb", cb, j)],
                    out=hb[:, cb, j, :], in0=xt[:, j, :], scalar=rsA[:, cb, j:j + 1], in1=gain1[:],
                    op0=ALU.mult, op1=ALU.mult)
            for kp in range(4):
                bank = nb % 2
                nb += 1
                for kk in range(2):
                    k = kp * 2 + kk
                    for j in range(4):
                        P.op("pe", "transpose", reads=[("hb", cb, j), "ident"], writes=[("bank", bank)],
                            out=bk_bf[bank][:, kk * 512 + j * 128: kk * 512 + (j + 1) * 128],
                            in_=hb[:, cb, j, k * 128:(k + 1) * 128], identity=ident[:])
                P.op("act", "copy", reads=[("bank", bank)], writes=[("hT", c)],
                    out=hT[:, kp * 2:kp * 2 + 2, c * 512:(c + 1) * 512],
                    in_=bk_bf[bank].rearrange("p (a b) -> p a b", a=2))
        P.barrier()
        sA.close()

        sB = ExitStack()
        sbB = lambda name, shape, dt: sB.enter_context(nc.sbuf_tensor(name, shape, dt))
        wh = sbB("wh", [128, 2, 8, 384], BF16)
        KT = sbB("KT", [128, 2, T], BF16)
        QT = sbB("QT", [128, 2, TO], BF16)
        VA = sbB("VA", [128, 2, NT, 130], BF16)
        raw = sbB("raw", [128, 1, 4, 384], F32)
        sqb = sbB("sqb", [128, 4, 256], F32)
        ssB = sbB("ssB", [128, 4, 4], F32)
        msB = sbB("msB", [128, 4, 4], F32)
        rsB = sbB("rsB", [128, 4, 4], F32)
        tA = sbB("tA", [128, 1, 4, 2, 32], F32)
        tB = sbB("tB", [128, 1, 4, 2, 32], F32)
        rot = sbB("rot", [128, 1, 4, 2, 64], F32)
        qkb = sbB("qkb", [128, 2, 4, 128], BF16)
        PT = sbB("PT", [128, 2, 2, 512], BF16)
        rcp = sbB("rcp", [128, 3, 3], F32)
        nrcp = sbB("nrcp", [128, 3, 3], F32)
        ocp = sbB("ocp", [128, 3, 387], F32)
        osq = sqb[:, :, 0:128]
        oss = sbB("oss", [128, 4], F32)
        oms = sbB("oms", [128, 4], F32)
        ors = sbB("ors", [128, 4], F32)
        onb = sbB("onb", [128, 4, 128], BF16)

        PREP = 7
        SB = [(0, 1), (2, 3)]
        OB = [4, 5, 6]
        pS = [None, None]
        for i in range(2):
            pass
        P.op("pool", "memset", writes=[("rcp", 2)], ap=rcp[:], constant=1.0)
        for hb_i in range(2):
            P.op("pool", "memset", writes=[("VA1", hb_i)], ap=VA[:, hb_i, :, 128:130], constant=1.0)

        def slot_ap(j, s):
            idx = j * 2 + s
            b, sl = OB[idx // 3], idx % 3
            return banks[b][:, sl * 129: sl * 129 + 129], b, idx

        def prep_start(h):
            hbi = h % 2
            P.dma("pool", writes=[("wh", hbi)], out=wh[:, hbi], in_=wqkv_d[h])

        def proj_tile(h, c, j):
            hbi = h % 2
            own = c < NCHO
            lo = 0 if own else 128
            rb = 0
            if True:
                if True:
                    tt = c * 4 + j
                    for k in range(8):
                        P.op("pe", "matmul", reads=[("hT", c), ("wh", hbi)], writes=[("bank", PREP)],
                            out=banks[PREP][:, lo:384], lhsT=hT[:, k, tt * 128:(tt + 1) * 128], rhs=wh[:, hbi, k, lo:384],
                            start=(k == 0), stop=(k == 7))
                    P.op("dve", "tensor_copy", reads=[("bank", PREP)], writes=[("raw", rb, j)], out=raw[:, rb, j, lo:384], in_=banks[PREP][:, lo:384])

        def elem(h, c):
            hbi = h % 2
            own = c < NCHO
            lo = 0 if own else 128
            rb = 0
            if True:
                rr = [("raw", rb, j) for j in range(4)]
                ng = 4 if own else 2
                qk = raw[:, rb, :, lo:256]
                P.op("pool", "tensor_tensor", reads=rr, writes=["sqb"], out=sqb[:, :, lo:256], in0=qk, in1=qk, op=ALU.mult)
                gl = lo // 64
                P.op("dve", "tensor_reduce", reads=["sqb"], writes=["ssB"],
                    out=ssB[:, :, gl:4], in_=sqb[:, :, lo:256].rearrange("p t (g d) -> p t g d", d=64), axis=AX.X, op=ALU.add)
                P.op("dve", "tensor_scalar", reads=["ssB"], writes=["msB"], out=msB[:, :, gl:4], in0=ssB[:, :, gl:4], scalar1=1.0 / 64, scalar2=1e-6,
                                                              op0=ALU.mult, op1=ALU.add)
                P.op("pool", "tensor_tensor", reads=["msB", "neghalf"], writes=["rsB"], out=rsB[:, :, gl:4], in0=msB[:, :, gl:4],
                                                               in1=neghalf[:, 0:4 * (4 - gl)].rearrange("p (a b) -> p a b", a=4), op=ALU.pow)
                for qi in ([0, 1] if own else [1]):
                    eng = "dve"
                    xv = raw[:, rb, :, qi * 128:(qi + 1) * 128].rearrange("p t (g d) -> p t g d", d=64)
                    x1 = xv[:, :, :, 0:32]
                    x2 = xv[:, :, :, 32:64]
                    Tt = TQ if qi == 0 else TK
                    tname = "TQ" if qi == 0 else "TK"

                    def tb(ti):
                        return Tt[:, c * 4:c * 4 + 4, ti, :].unsqueeze(2).to_broadcast([128, 4, 2, 32])
                    A, B = tA[:, 0], tB[:, 0]
                    P.op(eng, "tensor_tensor", reads=rr + [tname], writes=["tA"], out=A, in0=x1, in1=tb(0), op=ALU.mult)
                    P.op(eng, "tensor_tensor", reads=rr + [tname], writes=["tB"], out=B, in0=x2, in1=tb(1), op=ALU.mult)
                    P.op(eng, "tensor_tensor", reads=["tA", "tB"], writes=["rot1"], out=rot[:, 0, :, :, 0:32], in0=A, in1=B, op=ALU.subtract)
                    P.op(eng, "tensor_tensor", reads=rr + [tname, "rot1"], writes=["tA"], out=A, in0=x1, in1=tb(2), op=ALU.mult)
                    P.op(eng, "tensor_tensor", reads=rr + [tname, "rot1"], writes=["tB"], out=B, in0=x2, in1=tb(3), op=ALU.mult)
                    P.op(eng, "tensor_tensor", reads=["tA", "tB"], writes=["rot2"], out=rot[:, 0, :, :, 32:64], in0=A, in1=B, op=ALU.add)
                    P.op(eng, "tensor_tensor", reads=["rot1", "rot2", "rsB"], writes=[("qkb", qi)],
                        out=qkb[:, qi].rearrange("p t (g d) -> p t g d", d=64), in0=rot[:, 0],
                        in1=rsB[:, :, 2 * qi:2 * qi + 2].unsqueeze(3).to_broadcast([128, 4, 2, 64]), op=ALU.mult)
                P.op("pool", "tensor_copy", reads=rr, writes=[("VA", hbi, c)], out=VA[:, hbi, c * 4:c * 4 + 4, 0:128], in_=raw[:, rb, :, 256:384])

        def trans(h, c):
            hbi = h % 2
            own = c < NCHO
            if True:
                for qi in ([0, 1] if own else [1]):
                    for j in range(4):
                        P.op("pe", "transpose", reads=[("qkb", qi), "ident"], writes=[("bank", PREP)],
                            out=bk_bf[PREP][:, (qi * 4 + j) * 128:(qi * 4 + j + 1) * 128], in_=qkb[:, qi, j, :], identity=ident[:])
                P.op("dve", "tensor_copy", reads=[("bank", PREP)], writes=[("KT", hbi, c)], out=KT[:, hbi, c * 512:(c + 1) * 512], in_=bk_bf[PREP][:, 512:1024])
                if own:
                    P.op("dve", "tensor_copy", reads=[("bank", PREP)], writes=[("QT", hbi, c)], out=QT[:, hbi, c * 512:(c + 1) * 512], in_=bk_bf[PREP][:, 0:512])

        def attn(h, inject):
            hbi = h % 2
            it = 0
            tot = NCHO * NT
            every = max(1, tot // max(1, len(inject)))
            for qc in range(NCHO):
                def qk_mm(kt, sbi):
                    kc = kt // 4
                    for s in range(2):
                        P.op("pe", "matmul", reads=[("KT", hbi, kc), ("QT", hbi, qc)], writes=[("bank", SB[sbi][s])],
                            out=banks[SB[sbi][s]][:, :], lhsT=KT[s * 64:(s + 1) * 64, hbi, kt * 128:(kt + 1) * 128],
                            rhs=QT[s * 64:(s + 1) * 64, hbi, qc * 512:(qc + 1) * 512], start=True, stop=True)

                def exp_op(kt, sbi, pbi):
                    for s in range(2):
                        P.op("act", "activation", reads=[("bank", SB[sbi][s])], writes=[("PT", pbi, s)], out=PT[:, pbi, s, :], in_=banks[SB[sbi][s]][:, :], func=AF.Exp, scale=0.125)

                def pv_mm(kt, pbi):
                    kc = kt // 4
                    for s in range(2):
                        for j in range(4):
                            oap, b, idx = slot_ap(j, s)
                            first = (kt == 0) and ((j, s) in ((0, 0), (2, 0), (3, 0)))
                            P.op("pe", "matmul", reads=[("PT", pbi, s), ("VA", hbi, kc), ("VA1", hbi)], writes=[("bank", b)],
                                out=oap, lhsT=PT[:, pbi, s, j * 128:(j + 1) * 128], rhs=VA[:, hbi, kt, 0:129],
                                start=first, stop=(kt == NT - 1), skip_group_check=True)

                for kt in range(NT + 1):
                    if kt < NT:
                        qk_mm(kt, (it + kt) % 2)
                        exp_op(kt, (it + kt) % 2, (it + kt) % 2)
                    if kt >= 1:
                        pv_mm(kt - 1, (it + kt - 1) % 2)
                    if kt == min(8, NT - 1) and deferred:
                        deferred.pop(0)()
                    if kt < NT and (it + kt) % every == 0 and inject:
                        inject.pop(0)()
                it += NT
                ob = [("bank", b) for b in OB]
                for bi in range(3):
                    nsl = 3 if bi < 2 else 2
                    P.op("dve", "tensor_copy", reads=[ob[bi]], writes=[("ocp", bi)], out=ocp[:, bi, 0:129 * nsl], in_=banks[OB[bi]][:, 0:129 * nsl])
                for bi in range(3):
                    nsl = 3 if bi < 2 else 2
                    P.op("dve", "reciprocal", reads=[("ocp", bi)], writes=[("rcp", bi)], out=rcp[:, bi, 0:nsl],
                         in_=ocp[:, bi, 128:128 + 129 * (nsl - 1) + 1:129])
                P.op("dve", "tensor_scalar", reads=[("rcp", 0), ("rcp", 1), ("rcp", 2), "neglam"], writes=["nrcp"], out=nrcp[:], in0=rcp[:], scalar1=lam_t[:, 5:6], scalar2=None, op0=ALU.mult)

                def oslot(j, s_):
                    idx = j * 2 + s_
                    return ocp[:, idx // 3, (idx % 3) * 129:(idx % 3) * 129 + 128], idx
                for j in range(4):
                    o1, i1 = oslot(j, 0)
                    o2, i2 = oslot(j, 1)
                    P.op("dve", "tensor_scalar", reads=[("ocp", i1 // 3), ("rcp", i1 // 3)], writes=[("ocp", i1 // 3)],
                        out=o1, in0=o1, scalar1=rcp[:, i1 // 3, i1 % 3:i1 % 3 + 1], scalar2=None, op0=ALU.mult)
                    P.op("dve", "scalar_tensor_tensor", reads=[("ocp", i2 // 3), ("ocp", i1 // 3), "nrcp"], writes=[("ocp", i1 // 3)],
                        out=o1, in0=o2, scalar=nrcp[:, i2 // 3, i2 % 3:i2 % 3 + 1], in1=o1, op0=ALU.mult, op1=ALU.add)
                oc = [("ocp", bi) for bi in range(3)]
                for j in range(4):
                    o1, i1 = oslot(j, 0)
                    P.op("pool", "tensor_tensor", reads=oc, writes=["sqb"], out=osq[:, j, :], in0=o1, in1=o1, op=ALU.mult)
                P.op("dve", "tensor_reduce", reads=["sqb"], writes=["oss"], out=oss[:], in_=osq, axis=AX.X, op=ALU.add)
                P.op("dve", "tensor_scalar", reads=["oss"], writes=["oms"], out=oms[:], in0=oss[:], scalar1=1.0 / 128, scalar2=1e-6, op0=ALU.mult, op1=ALU.add)
                P.op("pool", "tensor_tensor", reads=["oms", "neghalf"], writes=["ors"], out=ors[:], in0=oms[:], in1=neghalf[:, 0:4], op=ALU.pow)
                for j in range(4):
                    o1, i1 = oslot(j, 0)
                    P.op("dve", "scalar_tensor_tensor", reads=oc + ["ors", "gsub"], writes=[("onb", j)],
                        out=onb[:, j, :], in0=o1, scalar=ors[:, j:j + 1], in1=gsub[:], op0=ALU.mult, op1=ALU.mult)

                def fin_o(h=h, qc=qc):
                    for j in range(4):
                        P.op("pe", "transpose", reads=[("onb", j), "ident"], writes=[("bank", PREP)], out=bk_bf[PREP][:, j * 128:(j + 1) * 128], in_=onb[:, j, :], identity=ident[:])
                    P.op("act", "copy", reads=[("bank", PREP)], writes=[("oT", h, qc)], out=oT[:, h, qc * 512:(qc + 1) * 512], in_=bk_bf[PREP][:, 0:512])
                deferred.append(fin_o)

        def prep_steps(h):
            steps = []
            for c in range(NCH):
                for j in range(4):
                    if j < 3:
                        steps.append(lambda c=c, j=j: proj_tile(h, c, j))
                    elif c > 0:
                        steps.append(lambda c=c, j=j: (trans(h, c - 1), proj_tile(h, c, j), elem(h, c)))
                    else:
                        steps.append(lambda c=c, j=j: (proj_tile(h, c, j), elem(h, c)))
            steps.append(lambda: trans(h, NCH - 1))
            return steps

        deferred = []
        prep_start(0)
        for f in prep_steps(0):
            f()
        for h in range(NH):
            inj = []
            if h + 1 < NH:
                prep_start(h + 1)
                inj = prep_steps(h + 1)
            attn(h, inj)
            while inj:
                inj.pop(0)()
            if h == NH - 1:
                while deferred:
                    deferred.pop(0)()
            if "KT" in dbg and h == NH - 1:
                hbi = h % 2
                fin.append(P.dma("sp", reads=[("KT", hbi, c) for c in range(NCH)], out=dbg["KT"], in_=KT[:, hbi, :]))
                fin.append(P.dma("sp", reads=[("QT", hbi, c) for c in range(NCHO)], out=dbg["QT"], in_=QT[:, hbi, :]))
                fin.append(P.dma("sp", reads=[("VA", hbi, c) for c in range(NCH)] + [("VA1", hbi)], out=dbg["V"], in_=VA[:, hbi]))
        if "oT" in dbg:
            fin.append(P.dma("sp", reads=[("oT", h, qc) for h in range(NH) for qc in range(NCHO)], out=dbg["oT"], in_=oT[:]))
        P.barrier()
        sB.close()
        sT.close()

        P.dma("sp", writes=["cp"], out=cp[:], in_=cp_d)
        P.op("dve", "tensor_scalar", reads=["cp"], writes=["hbias"], out=hbias[:], in0=cp[:, :, 6:10], scalar1=0.5, scalar2=None, op0=ALU.mult)
        P.op("act", "activation", reads=["cp"], writes=["lrt"], out=lrt[:], in_=cp[:, :, 10:12], func=AF.Exp, scale=-1.0)
        P.op("act", "activation", reads=["lrt"], writes=["lrt2"], out=lrt[:], in_=lrt[:], func=AF.Ln, bias=1.0)
        P.op("dve", "tensor_scalar", reads=["lrt2"], writes=["hc"], out=hc[:], in0=lrt[:], scalar1=-4.0, scalar2=None, op0=ALU.mult)
        yT = sb("yT", [128, 8, TO], BF16)
        sL = ExitStack()
        sbL = lambda name, shape, dt: sL.enter_context(nc.sbuf_tensor(name, shape, dt))
        wl = sbL("wl_s", [128, 2, 2, 8, 128], BF16)
        wgt = sbL("wgt", [128, 2, 4, 128], BF16)
        xs = sbL("xs", [128, TO + 4], F32)
        xr = sbL("xr", [128, TO], F32)
        xrb = sbL("xrb", [128, TO], BF16)
        Ab = sbL("Ab", [128, TO], F32)
        Ub = sbL("Ub", [128, TO], F32)
        T1 = sbL("T1", [128, TO], F32)
        hA = sbL("hA", [128, TO], F32)
        gg = sbL("gg", [128, TO], F32)
        stB = sbL("stB", [128, 1], F32)
        NCL = TO // 512
        cnt = {"p": 0, "g": 0}
        g2 = T1

        def ck(name):
            return [(name, ch) for ch in range(NCL)]

        def proj_pieces(widx, buf, tok_lo, ntok, dst, dst_col, dkeys, evac_eng):
            pieces = []
            done = 0
            while done < ntok:
                w = min(512, ntok - done)

                def piece(done=done, w=w):
                    bank = cnt["p"] % 4
                    cnt["p"] += 1
                    lo = tok_lo + done
                    for k in range(8):
                        P.op("pe", "matmul", reads=[("hT", lo // 512), ("hT", (lo + w - 1) // 512), ("wl", buf, widx)], writes=[("bank", bank)],
                             out=banks[bank][:, 0:w], lhsT=wl[:, buf, widx, k, :], rhs=hT[:, k, lo:lo + w], start=(k == 0), stop=(k == 7))
                    if evac_eng == "act":
                        P.op("act", "copy", reads=[("bank", bank)], writes=dkeys(dst_col + done, dst_col + done + w), out=dst[:, dst_col + done:dst_col + done + w], in_=banks[bank][:, 0:w])
                    else:
                        P.op("dve", "tensor_copy", reads=[("bank", bank)], writes=dkeys(dst_col + done, dst_col + done + w), out=dst[:, dst_col + done:dst_col + done + w], in_=banks[bank][:, 0:w])
                pieces.append(piece)
                done += w
            return pieces

        def xskeys(c0, c1):
            return [("xs", i) for i in range(c0 // 512, (c1 - 1) // 512 + 1)]

        def ggkeys(c0, c1):
            return [("gg", i) for i in range(c0 // 512, (c1 - 1) // 512 + 1)]

        def conv_chunk(n, ch):
            lo = ch * 512
            xk = xskeys(lo, lo + 516)
            P.op("dve", "tensor_scalar", reads=xk + ["cp"], writes=[("xr", ch)], out=xr[:, lo:lo + 512], in0=xs[:, lo:lo + 512], scalar1=cp[:, n, 0:1], scalar2=cp[:, n, 5:6],
                 op0=ALU.mult, op1=ALU.add)
            for j in range(1, 5):
                P.op("dve", "scalar_tensor_tensor", reads=xk + ["cp", ("xr", ch)], writes=[("xr", ch)], out=xr[:, lo:lo + 512], in0=xs[:, lo + j:lo + j + 512], scalar=cp[:, n, j:j + 1],
                     in1=xr[:, lo:lo + 512], op0=ALU.mult, op1=ALU.add)
            P.op("pool", "tensor_copy", reads=[("xr", ch)], writes=[("xrb", ch)], out=xrb[:, lo:lo + 512], in_=xr[:, lo:lo + 512])

        def front_end(n, phase):
            buf = n % 2
            if phase == "O":
                first = [lambda: P.op("pool", "memset", reads=[], writes=xskeys(TO + 2, TO + 4), ap=xs[:, TO + 2:TO + 4], constant=0.0)]
                pcs = proj_pieces(0, buf, TO - 2, TO + 2, xs, 0, xskeys, "dve")
            else:
                first = [lambda: P.op("pool", "memset", reads=[], writes=xskeys(0, 2), ap=xs[:, 0:2], constant=0.0)]
                pcs = proj_pieces(0, buf, 0, TO + 2, xs, 2, xskeys, "dve")
            pcs = first + pcs
            state = {"i": 0}

            def hook(ch):
                need = len(pcs) if ch == NCL - 1 else min(len(pcs), ch + 3)
                while state["i"] < need:
                    pcs[state["i"]]()
                    state["i"] += 1
                conv_chunk(n, ch)
            return hook

        def lru_dir(n, buf, d, reverse, init, init_reads, out, okey, hook=None):
            def gates(ch):
                sl = slice(ch * 512, (ch + 1) * 512)
                bx = 4 + cnt["g"] % 4
                by = 4 + (cnt["g"] + 1) % 4
                cnt["g"] += 2
                P.op("pe", "matmul", reads=[("xrb", ch), ("wgt", buf)], writes=[("bank", bx)], out=banks[bx][:, :], lhsT=wgt[:, buf, 2 * d, :], rhs=xrb[:, sl], start=True, stop=True)
                P.op("pe", "matmul", reads=[("xrb", ch), ("wgt", buf)], writes=[("bank", by)], out=banks[by][:, :], lhsT=wgt[:, buf, 2 * d + 1, :], rhs=xrb[:, sl], start=True, stop=True)
                return bx, by
            nxt = gates(0)
            for ch in range(NCL):
                sl = slice(ch * 512, (ch + 1) * 512)
                bx, by = nxt
                P.op("act", "activation", reads=[("bank", bx), "hbias"], writes=[("T1", ch)], out=T1[:, sl], in_=banks[bx][:, :], func=AF.Tanh, scale=0.5, bias=hbias[:, n, d:d + 1])
                P.op("act", "activation", reads=[("bank", by), "hbias"], writes=[("U", ch)], out=Ub[:, sl], in_=banks[by][:, :], func=AF.Tanh, scale=0.5, bias=hbias[:, n, 2 + d:3 + d])
                P.op("act", "activation", reads=[("T1", ch), "hc"], writes=[("A", ch)], out=Ab[:, sl], in_=T1[:, sl], func=AF.Exp, scale=hc[:, n, d:d + 1], bias=hc[:, n, d:d + 1])
                if ch + 1 < NCL:
                    nxt = gates(ch + 1)
                P.op("pool", "tensor_scalar", reads=[("A", ch)], writes=[("A", ch)], out=Ab[:, sl], in0=Ab[:, sl], scalar1=1.0, scalar2=-3.0e38, op0=ALU.min, op1=ALU.max)
                P.op("pool", "tensor_tensor", reads=[("A", ch)], writes=[("T1", ch)], out=T1[:, sl], in0=Ab[:, sl], in1=Ab[:, sl], op=ALU.mult)
                P.op("dve", "scalar_tensor_tensor", reads=[("U", ch), ("xr", ch)], writes=[("U", ch)], out=Ub[:, sl], in0=Ub[:, sl], scalar=1.0, in1=xr[:, sl], op0=ALU.add, op1=ALU.mult)
                if hook is not None:
                    hook(ch)
            P.op("act", "activation", reads=ck("T1"), writes=ck("T1"), out=T1[:], in_=T1[:], func=AF.Sqrt, scale=-1.0, bias=1.0)
            P.op("dve", "scalar_tensor_tensor", reads=ck("U") + ck("T1"), writes=ck("U"), out=Ub[:], in0=Ub[:], scalar=0.5, in1=T1[:], op0=ALU.mult, op1=ALU.mult)
            if reverse:
                P.op("dve", "tensor_tensor_scan", reads=ck("A") + ck("U") + init_reads, writes=okey, out=out[:, ::-1], data0=Ab[:, ::-1], data1=Ub[:, ::-1], initial=init,
                     op0=ALU.mult, op1=ALU.add)
            else:
                P.op("dve", "tensor_tensor_scan", reads=ck("A") + ck("U") + init_reads, writes=okey, out=out[:], data0=Ab[:], data1=Ub[:], initial=init, op0=ALU.mult, op1=ALU.add)

        def l_weights(n):
            buf = n % 2
            P.dma("pool", writes=[("wl", buf, 0)], out=wl[:, buf, 0], in_=wlx_d[n])
            P.dma("pool", writes=[("wl", buf, 1)], out=wl[:, buf, 1], in_=wlg_d[n])
            P.dma("pool", writes=[("wgt", buf)], out=wgt[:, buf], in_=wg_d[n])

        def l_mid(n):
            buf = n % 2
            if n + 1 < NB:
                l_weights(n + 1)
            lru_dir(n, buf, 1, True, 0.0, [], Ab, ck("A"), hook=front_end(n, "W"))
            P.op("dve", "tensor_copy", reads=ck("A"), writes=["stB"], out=stB[:], in_=Ab[:, 0:1])
            gpcs = proj_pieces(1, buf, 0, TO, gg, 0, ggkeys, "dve")
            lru_dir(n, buf, 0, False, 0.0, [], hA, ["hA"], hook=lambda ch: gpcs[ch]() if ch < len(gpcs) else None)
            for f in gpcs[NCL:]:
                f()
            nxt = front_end(n + 1, "O") if n + 1 < NB else None
            lru_dir(n, buf, 1, True, stB[:, 0:1], ["stB"], Ab, ck("A"), hook=nxt)

        def l_tail(n):
            P.op("pool", "tensor_tensor", reads=["hA"] + ck("A"), writes=["hA"], out=hA[:], in0=hA[:], in1=Ab[:], op=ALU.add)
            P.op("pool", "tensor_tensor", reads=ck("gg"), writes=ck("T1"), out=g2[:], in0=gg[:], in1=gg[:], op=ALU.mult)
            P.op("pool", "tensor_scalar", reads=ck("T1"), writes=ck("T1"), out=g2[:], in0=g2[:], scalar1=0.044715, scalar2=1.0, op0=ALU.mult, op1=ALU.add)
            P.op("dve", "tensor_tensor", reads=ck("T1") + ck("gg"), writes=ck("T1"), out=g2[:], in0=g2[:], in1=gg[:], op=ALU.mult)
            P.op("act", "activation", reads=ck("T1"), writes=ck("T1"), out=g2[:], in_=g2[:], func=AF.Tanh, scale=0.7978845608028654)
            P.op("dve", "scalar_tensor_tensor", reads=ck("T1") + ck("gg"), writes=ck("T1"), out=g2[:], in0=g2[:], scalar=1.0, in1=gg[:], op0=ALU.add, op1=ALU.mult)
            P.op("dve", "scalar_tensor_tensor", reads=["hA"] + ck("T1"), writes=[("yT", n)], out=yT[:, n, :], in0=hA[:], scalar=0.5, in1=g2[:], op0=ALU.mult, op1=ALU.mult)

        l_weights(0)
        h0 = front_end(0, "O")
        for ch in range(NCL):
            h0(ch)
        for n in range(NB):
            l_mid(n)
            l_tail(n)
        if "yT" in dbg:
            fin.append(P.dma("sp", reads=[("yT", n) for n in range(NB)], out=dbg["yT"], in_=yT[:]))
        P.barrier()
        sL.close()

        sM = ExitStack()
        sbM = lambda name, shape, dt: sM.enter_context(nc.sbuf_tensor(name, shape, dt))
        mT = sbM("mT", [128, 8, TO], BF16)
        cw = sbM("cw", [128, NTO, 16], F32)
        bg = sbM("bg_s", [128, 16], F32)
        gain2 = sbM("gain2", [128, D], F32)
        wr32 = sbM("wr32", [128, 8, 20], F32)
        wrb = sbM("wrb", [128, 8, 20], BF16)
        brb = sbM("brb", [128, 20], F32)
        sM1 = ExitStack()
        sbM1 = lambda name, shape, dt: sM1.enter_context(nc.sbuf_tensor(name, shape, dt))
        wm = sbM1("wm_s", [128, 2, 4, 8, 128], BF16)
        gsb = sbM1("gsb", [128, 2, 2, 512], F32)
        P.dma("sp", writes=["bg"], out=bg[:], in_=bg_d)
        P.dma("sp", writes=["gain2"], out=gain2[:], in_=g2_d.partition_broadcast(128))
        P.dma("sp", writes=["wr32"], out=wr32[:], in_=wr_d)
        P.dma("sp", writes=["brb"], out=brb[:], in_=br_d.partition_broadcast(128))
        P.op("pool", "tensor_copy", reads=["wr32"], writes=["wrb"], out=wrb[:], in_=wr32[:])
        it = 0
        for ec in range(8):
            buf = ec % 2
            for wi_, wd_ in enumerate((wao_d, wlo_d, wga_d, wgl_d)):
                P.dma("pool", writes=[("wm", buf, wi_)], out=wm[:, buf, wi_], in_=wd_[ec])
            for tc in range(NCHO):
                st_ = it % 2
                it += 1
                bb = [4 * st_ + i for i in range(4)]
                tsl = slice(tc * 512, (tc + 1) * 512)
                srcs = [(oT, [("oT", h, tc) for h in range(NH)]), (yT, [("yT", n) for n in range(NB)]), (hT, [("hT", tc)]), (hT, [("hT", tc)])]
                for wi_ in range(4):
                    src, rk = srcs[wi_]
                    for k in range(8):
                        P.op("pe", "matmul", reads=rk + [("wm", buf, wi_)], writes=[("bank", bb[wi_])], out=banks[bb[wi_]][:, :],
                             lhsT=wm[:, buf, wi_, k, :], rhs=src[:, k, tsl], start=(k == 0), stop=(k == 7))
                P.op("act", "activation", reads=[("bank", bb[2]), "bg"], writes=[("gsb", st_, 0)], out=gsb[:, st_, 0, :], in_=banks[bb[2]][:, :], func=AF.Sigmoid, bias=bg[:, ec:ec + 1])
                P.op("act", "activation", reads=[("bank", bb[3]), "bg"], writes=[("gsb", st_, 1)], out=gsb[:, st_, 1, :], in_=banks[bb[3]][:, :], func=AF.Sigmoid, bias=bg[:, 8 + ec:9 + ec])
                P.op("dve", "tensor_tensor", reads=[("gsb", st_, 0), ("bank", bb[0])], writes=[("gsb", st_, 0)], out=gsb[:, st_, 0, :], in0=gsb[:, st_, 0, :], in1=banks[bb[0]][:, :], op=ALU.mult)
                P.op("dve", "tensor_tensor", reads=[("gsb", st_, 1), ("bank", bb[1])], writes=[("gsb", st_, 1)], out=gsb[:, st_, 1, :], in0=gsb[:, st_, 1, :], in1=banks[bb[1]][:, :], op=ALU.mult)
                P.op("dve", "tensor_tensor", reads=[("gsb", st_, 0), ("gsb", st_, 1)], writes=[("mT", ec, tc)], out=mT[:, ec, tsl], in0=gsb[:, st_, 0, :], in1=gsb[:, st_, 1, :], op=ALU.add)
        P.barrier()
        sM1.close()
        x1 = hT[:].rearrange("p a b -> p (a b)").bitcast(F32).rearrange("p (a b) -> p a b", b=D)
        h2T = oT
        sM2 = ExitStack()
        sbM2 = lambda name, shape, dt: sM2.enter_context(nc.sbuf_tensor(name, shape, dt))
        wo = sbM2("wo_s", [128, 8, D], BF16)
        xt2 = sbM2("xt2", [128, 2, D], F32)
        junk2 = sbM2("junk2", [128, D], BF16)
        ss2 = sbM2("ss2", [128, 4], F32)
        ms2 = sbM2("ms2", [128, 4], F32)
        rs2 = sbM2("rs2", [128, 4], F32)
        h2b = sbM2("h2b", [128, 4, D], BF16)
        lg = sbM2("lg", [128, 20], F32)
        rt = sbM2("rt", [128, 352], F32)
        P.dma("pool", writes=["wo"], out=wo[:], in_=wout_d)
        for c in range(NCHO):
            for j in range(4):
                tt = c * 4 + j
                xb = tt % 2
                P.dma("sp", writes=[("xt2", xb)], out=xt2[:, xb, :], in_=x_d[tt * 128:(tt + 1) * 128, :])
                for half in range(2):
                    bank = (tt * 2 + half) % 4
                    for ec in range(8):
                        P.op("pe", "matmul", reads=[("mT", ec, c), "wo"], writes=[("bank", bank)], out=banks[bank][:, :],
                             lhsT=mT[:, ec, tt * 128:(tt + 1) * 128], rhs=wo[:, ec, half * 512:(half + 1) * 512], start=(ec == 0), stop=(ec == 7))
                    P.op("dve", "tensor_tensor", reads=[("bank", bank), ("xt2", xb)], writes=[("x1", tt)], out=x1[:, tt, half * 512:(half + 1) * 512],
                         in0=xt2[:, xb, half * 512:(half + 1) * 512], in1=banks[bank][:, :], op=ALU.add)
                P.op("act", "activation", reads=[("x1", tt)], writes=["junk2", ("ss2", j)], out=junk2[:], in_=x1[:, tt, :], func=AF.Square, accum_out=ss2[:, j:j + 1])
            P.op("dve", "tensor_scalar", reads=[("ss2", j) for j in range(4)], writes=["ms2"], out=ms2[:], in0=ss2[:], scalar1=1.0 / D, scalar2=1e-6, op0=ALU.mult, op1=ALU.add)
            P.op("pool", "tensor_tensor", reads=["ms2", "neghalf"], writes=["rs2"], out=rs2[:], in0=ms2[:], in1=neghalf[:, 0:4], op=ALU.pow)
            for j in range(4):
                tt = c * 4 + j
                P.op("dve", "scalar_tensor_tensor", reads=[("x1", tt), "rs2", "gain2"], writes=[("h2b", j)], out=h2b[:, j, :], in0=x1[:, tt, :], scalar=rs2[:, j:j + 1], in1=gain2[:],
                     op0=ALU.mult, op1=ALU.mult)
            for kp in range(4):
                bank = 4 + kp % 2
                for kk in range(2):
                    k = kp * 2 + kk
                    for j in range(4):
                        P.op("pe", "transpose", reads=[("h2b", j), "ident"], writes=[("bank", bank)],
                             out=bk_bf[bank][:, kk * 512 + j * 128: kk * 512 + (j + 1) * 128], in_=h2b[:, j, k * 128:(k + 1) * 128], identity=ident[:])
                P.op("act", "copy", reads=[("bank", bank)], writes=[("h2T", c)], out=h2T[:, kp * 2:kp * 2 + 2, c * 512:(c + 1) * 512],
                     in_=bk_bf[bank].rearrange("p (a b) -> p a b", a=2))
            for j in range(4):
                tt = c * 4 + j
                for k in range(8):
                    P.op("pe", "matmul", reads=[("h2T", c), "wrb"], writes=[("bank", 6)], out=banks[6][:, j * 20:(j + 1) * 20], lhsT=h2T[:, k, tt * 128:(tt + 1) * 128], rhs=wrb[:, k, :],
                         start=(k == 0), stop=(k == 7), skip_group_check=True)
            lg4 = rt[:, 0:80].rearrange("p (t f) -> p t f", f=20)
            P.op("dve", "tensor_tensor", reads=[("bank", 6), "brb"], writes=["lg4"], out=lg4, in0=banks[6][:, 0:80].rearrange("p (t f) -> p t f", f=20),
                 in1=brb[:].unsqueeze(1).to_broadcast([128, 4, 20]), op=ALU.add)
            glog = lg4[:, :, 0:4]
            elog = lg4[:, :, 4:20].rearrange("p t (g e) -> p t g e", e=4)
            V = lambda o, n: rt[:, o:o + n]
            V3 = lambda o: rt[:, o:o + 16].rearrange("p (t e) -> p t e", e=4)
            bc3 = lambda ap4: ap4.unsqueeze(2).to_broadcast([128, 4, 4])
            gmax, gsh, gexp, gsum, gw, goh = V(80, 4), V3(84), V3(100), V(116, 4), V(120, 4), V3(124)
            tmp4 = rt[:, 140:204].rearrange("p (t g e) -> p t g e", g=4, e=4)
            sel, m1, oh1, sel2, m2, oh2 = V3(204), V(220, 4), V3(224), V3(240), V(256, 4), V3(260)
            dd, e2, den, w1, w2, t1, t2, cwe = V(276, 4), V(280, 4), V(284, 4), V(288, 4), V(292, 4), V3(296), V3(312), V3(328)
            P.op("dve", "tensor_reduce", reads=["lg4"], writes=["gmax"], out=gmax, in_=glog, axis=AX.X, op=ALU.max)
            P.op("dve", "tensor_tensor", reads=["lg4", "gmax"], writes=["goh"], out=goh, in0=glog, in1=bc3(gmax), op=ALU.is_equal)
            P.op("dve", "tensor_tensor", reads=["lg4", "gmax"], writes=["gsh"], out=gsh, in0=glog, in1=bc3(gmax), op=ALU.subtract)
            P.op("act", "activation", reads=["gsh"], writes=["gexp"], out=gexp, in_=gsh, func=AF.Exp)
            P.op("dve", "tensor_reduce", reads=["gexp"], writes=["gsum"], out=gsum, in_=gexp, axis=AX.X, op=ALU.add)
            P.op("dve", "reciprocal", reads=["gsum"], writes=["gw"], out=gw, in_=gsum)
            P.op("dve", "tensor_tensor", reads=["lg4", "goh"], writes=["tmp4"], out=tmp4, in0=elog, in1=goh.unsqueeze(3).to_broadcast([128, 4, 4, 4]), op=ALU.mult)
            P.op("dve", "tensor_reduce", reads=["tmp4"], writes=["sel"], out=sel, in_=tmp4.rearrange("p t g e -> p t e g"), axis=AX.X, op=ALU.add)
            P.op("dve", "tensor_reduce", reads=["sel"], writes=["m1"], out=m1, in_=sel, axis=AX.X, op=ALU.max)
            P.op("dve", "tensor_tensor", reads=["sel", "m1"], writes=["oh1"], out=oh1, in0=sel, in1=bc3(m1), op=ALU.is_equal)
            P.op("dve", "scalar_tensor_tensor", reads=["oh1", "sel"], writes=["sel2"], out=sel2, in0=oh1, scalar=-1e30, in1=sel, op0=ALU.mult, op1=ALU.add)
            P.op("dve", "tensor_reduce", reads=["sel2"], writes=["m2"], out=m2, in_=sel2, axis=AX.X, op=ALU.max)
            P.op("dve", "tensor_tensor", reads=["sel2", "m2"], writes=["oh2"], out=oh2, in0=sel2, in1=bc3(m2), op=ALU.is_equal)
            P.op("dve", "tensor_tensor", reads=["m2", "m1"], writes=["dd"], out=dd, in0=m2, in1=m1, op=ALU.subtract)
            P.op("act", "activation", reads=["dd"], writes=["e2"], out=e2, in_=dd, func=AF.Exp)
            P.op("dve", "tensor_scalar", reads=["e2"], writes=["den"], out=den, in0=e2, scalar1=1.0, scalar2=None, op0=ALU.add)
            P.op("dve", "reciprocal", reads=["den"], writes=["w1a"], out=w1, in_=den)
            P.op("dve", "tensor_tensor", reads=["w1a", "gw"], writes=["w1"], out=w1, in0=w1, in1=gw, op=ALU.mult)
            P.op("dve", "tensor_tensor", reads=["w1", "e2"], writes=["w2"], out=w2, in0=w1, in1=e2, op=ALU.mult)
            P.op("dve", "tensor_tensor", reads=["oh1", "w1"], writes=["t1"], out=t1, in0=oh1, in1=bc3(w1), op=ALU.mult)
            P.op("dve", "tensor_tensor", reads=["oh2", "w2"], writes=["t2"], out=t2, in0=oh2, in1=bc3(w2), op=ALU.mult)
            P.op("dve", "tensor_tensor", reads=["t1", "t2"], writes=["cwe"], out=cwe, in0=t1, in1=t2, op=ALU.add)
            P.op("dve", "tensor_tensor", reads=["goh", "cwe"], writes=[("cw", c * 4 + j) for j in range(4)],
                 out=cw[:, c * 4:c * 4 + 4, :].rearrange("p t (g e) -> p t g e", e=4),
                 in0=goh.unsqueeze(3).to_broadcast([128, 4, 4, 4]), in1=cwe.unsqueeze(2).to_broadcast([128, 4, 4, 4]), op=ALU.mult)
        P.barrier()
        sM2.close()

        sE = ExitStack()
        sbE = lambda name, shape, dt: sE.enter_context(nc.sbuf_tensor(name, shape, dt))
        if TO >= 2048:
            yflat = yT[:].rearrange("p a b -> p (a b)")
            mflat = mT[:].rearrange("p a b -> p (a b)")
            weg = yflat[:, 0:8192].rearrange("p (u k f) -> p u k f", u=2, k=8)
            weu = yflat[:, 8192:16384].rearrange("p (u k f) -> p u k f", u=2, k=8)
            wed = mflat[:, 0:8192].rearrange("p (u f d) -> p u f d", u=2, f=4)
            hid = mflat[:, 8192:12288].rearrange("p (u f t) -> p u f t", u=2, f=4)
        else:
            weg = sbE("weg_s", [128, 2, 8, 512], BF16)
            weu = sbE("weu_s", [128, 2, 8, 512], BF16)
            wed = sbE("wed_s", [128, 2, 4, D], BF16)
            hid = sbE("hid", [128, 2, 4, 512], BF16)
        sg = sbE("sg", [128, 2, 512], F32)
        itE = 0
        for e_ in range(NE):
            buf = e_ % 2
            P.dma("pool", writes=[("weg", buf)], out=weg[:, buf], in_=weg_d[e_])
            P.dma("pool", writes=[("weu", buf)], out=weu[:, buf], in_=weu_d[e_])
            P.dma("pool", writes=[("wed", buf)], out=wed[:, buf], in_=wed_d[e_])
            for tc in range(NCHO):
                hb_ = itE % 2
                itE += 1
                tsl = slice(tc * 512, (tc + 1) * 512)
                for fc in range(4):
                    gb = (fc % 2) * 2
                    ub = gb + 1
                    for k in range(8):
                        P.op("pe", "matmul", reads=[("h2T", tc), ("weg", buf)], writes=[("bank", gb)], out=banks[gb][:, :],
                             lhsT=weg[:, buf, k, fc * 128:(fc + 1) * 128], rhs=h2T[:, k, tsl], start=(k == 0), stop=(k == 7))
                    for k in range(8):
                        P.op("pe", "matmul", reads=[("h2T", tc), ("weu", buf)], writes=[("bank", ub)], out=banks[ub][:, :],
                             lhsT=weu[:, buf, k, fc * 128:(fc + 1) * 128], rhs=h2T[:, k, tsl], start=(k == 0), stop=(k == 7))
                    P.op("act", "activation", reads=[("bank", gb)], writes=[("sg", fc % 2)], out=sg[:, fc % 2, :], in_=banks[gb][:, :], func=AF.Silu)
                    P.op("dve", "tensor_tensor", reads=[("sg", fc % 2), ("bank", ub)], writes=[("hid", hb_, fc)], out=hid[:, hb_, fc, :], in0=sg[:, fc % 2, :], in1=banks[ub][:, :], op=ALU.mult)
                for tj in range(4):
                    tt = tc * 4 + tj
                    for half in range(2):
                        db = 4 + (tj * 2 + half) % 4
                        for fc in range(4):
                            P.op("pe", "matmul", reads=[("hid", hb_, fc), ("wed", buf)], writes=[("bank", db)], out=banks[db][:, :],
                                 lhsT=hid[:, hb_, fc, tj * 128:(tj + 1) * 128], rhs=wed[:, buf, fc, half * 512:(half + 1) * 512], start=(fc == 0), stop=(fc == 3))
                        P.op("dve", "scalar_tensor_tensor", reads=[("bank", db), ("cw", tt), ("x1", tt)], writes=[("x1", tt)], out=x1[:, tt, half * 512:(half + 1) * 512],
                             in0=banks[db][:, :], scalar=cw[:, tt, e_:e_ + 1], in1=x1[:, tt, half * 512:(half + 1) * 512], op0=ALU.mult, op1=ALU.add)
                    if e_ == NE - 1:
                        fin.append(P.dma("sp", reads=[("x1", tt)], out=out_d[tt * 128:(tt + 1) * 128, :], in_=x1[:, tt, :]))
        sE.close()
        sM.close()

        stats = P.emit(nc, st, final_wait_ops=fin)
        print("stats", stats)
    return nc


_NC_CACHE = {}


def _blk(W):
    return np.ascontiguousarray(W.reshape(8, 128, 8, 128).transpose(2, 1, 0, 3))


def _pc(v):
    return v.reshape(8, 128).T


def make_in_maps(inp, T):
    f32 = np.float32
    B = inp["x"].shape[0]
    w_in = np.asarray(inp["w_in"][0], f32)
    inv = 10000.0 ** (-np.arange(0, 64, 2, dtype=np.float64) / 64.0)
    pos = np.arange(T, dtype=np.float64)
    ang = pos[:, None] * inv[None, :]
    cs_f = np.concatenate([np.cos(ang), np.sin(ang)], 1).astype(f32)
    wqkv = np.zeros((8, 128, 8, 384), f32)
    for i in range(3):
        Wp = w_in[:, i * 1024:(i + 1) * 1024].reshape(8, 128, 8, 128)
        wqkv[:, :, :, i * 128:(i + 1) * 128] = Wp.transpose(2, 1, 0, 3)
    shared = {
        "g1": np.asarray(inp["norm1_gain"][0][None, :], f32),
        "gq": np.asarray(inp["q_norm_gain"][0][None, :], f32),
        "gk": np.asarray(inp["k_norm_gain"][0][None, :], f32),
        "lamv": np.stack([inp["lambda_q1"][0], inp["lambda_k1"][0], inp["lambda_q2"][0], inp["lambda_k2"][0]]).astype(f32),
        "gsub": np.asarray(inp["attn_subln_gain"][0][None, :], f32),
        "wqkv": wqkv,
        "wlx": _blk(w_in[:, 3072:4096]), "wlg": _blk(w_in[:, 4096:5120]),
        "wga": _blk(w_in[:, 5120:6144]), "wgl": _blk(w_in[:, 6144:7168]),
        "wao": _blk(np.asarray(inp["w_attn_o"][0], f32)), "wlo": _blk(np.asarray(inp["w_lru_o"][0], f32)),
        "bg": np.ascontiguousarray(np.asarray(inp["b_gates"][0], f32).reshape(16, 128).T),
        "wout": np.ascontiguousarray(np.asarray(inp["w_out"][0], f32).reshape(8, 128, 1024).transpose(1, 0, 2)),
        "g2": np.asarray(inp["norm2_gain"][0][None, :], f32),
        "wr": np.ascontiguousarray(np.concatenate([inp["w_group_router"][0], inp["w_expert_router"][0]], 1).astype(f32).reshape(8, 128, 20).transpose(1, 0, 2)),
        "br": np.concatenate([inp["b_group_router"][0], inp["b_expert_router"][0]]).astype(f32)[None, :],
        "weg": np.ascontiguousarray(np.asarray(inp["w_expert_gate"][0], f32).reshape(16, 8, 128, 512).transpose(0, 2, 1, 3)),
        "weu": np.ascontiguousarray(np.asarray(inp["w_expert_up"][0], f32).reshape(16, 8, 128, 512).transpose(0, 2, 1, 3)),
        "wed": np.ascontiguousarray(np.asarray(inp["w_expert_down"][0], f32).reshape(16, 4, 128, 1024).transpose(0, 2, 1, 3)),
    }
    conv_w = np.asarray(inp["conv_w"][0], f32)
    conv_b = np.asarray(inp["conv_b"][0], f32)
    wa = np.asarray(inp["lru_wa"][0], f32); wi = np.asarray(inp["lru_wi"][0], f32)
    ba = np.asarray(inp["lru_ba"][0], f32); bi = np.asarray(inp["lru_bi"][0], f32)
    lam = np.asarray(inp["lru_lambda"][0], f32)
    per_half = []
    for half in range(2):
        dA, dB = (0, 1) if half == 0 else (1, 0)
        cp = np.zeros((128, 8, 12), f32)
        taps = [None, conv_w[0], conv_w[1], conv_w[2], conv_w[3]] if half == 0 else [conv_w[3], conv_w[2], conv_w[1], conv_w[0], None]
        for j, tp in enumerate(taps):
            if tp is not None:
                cp[:, :, j] = _pc(tp)
        cp[:, :, 5] = _pc(conv_b)
        cp[:, :, 6] = _pc(ba[dA]); cp[:, :, 7] = _pc(ba[dB]); cp[:, :, 8] = _pc(bi[dA]); cp[:, :, 9] = _pc(bi[dB])
        cp[:, :, 10] = _pc(lam[dA]); cp[:, :, 11] = _pc(lam[dB])
        wg = np.zeros((8, 128, 4, 128), f32)
        for n in range(8):
            wg[n, :, 0] = wa[dA, n]; wg[n, :, 1] = wi[dA, n]; wg[n, :, 2] = wa[dB, n]; wg[n, :, 3] = wi[dB, n]
        cs = cs_f if half == 0 else np.ascontiguousarray(cs_f[::-1])
        per_half.append({"cp": cp, "wg": wg, "cs": cs})
    maps = []
    for c in range(2 * B):
        b, half = c // 2, c % 2
        xb = np.asarray(inp["x"][b], f32)
        m = dict(shared)
        m.update(per_half[half])
        m["x"] = xb if half == 0 else np.ascontiguousarray(xb[::-1])
        maps.append(m)
    return maps


def assemble(results, B, T):
    TO = T // 2
    out = np.zeros((B, T, 1024), np.float32)
    for c in range(2 * B):
        b, half = c // 2, c % 2
        r = np.asarray(results[c]["out"], np.float32)
        if half == 0:
            out[b, :TO] = r
        else:
            out[b, TO:] = r[::-1]
    return out


def kernel(**inputs):
    T = inputs["x"].shape[1]
    B = inputs["x"].shape[0]
    key = (T,)
    if key not in _NC_CACHE:
        _NC_CACHE[key] = build(Cfg(T=T))
    nc = _NC_CACHE[key]
    maps = make_in_maps(inputs, T)
    res = run_bass_kernel_spmd(nc, maps, core_ids=list(range(2 * B)))
    return assemble(res.results, B, T)
```

```python
import math
import numpy as np
from contextlib import ExitStack
import concourse.bass as bass
import concourse.mybir as mybir
from concourse.bass_utils import run_bass_kernel_spmd


F32 = mybir.dt.float32
BF16 = mybir.dt.bfloat16
AF = mybir.ActivationFunctionType
ALU = mybir.AluOpType
AX = mybir.AxisListType

EPOCH = 16000
NDMASEM = 8


class Op:
    __slots__ = ("eng", "fn", "deps", "sig", "sigcount", "dma", "dsem", "dval", "dprev", "gi")


class Prog:
    ENG = ["pe", "act", "dve", "pool", "sp"]

    def __init__(self):
        self.q = {e: [] for e in self.ENG}
        self.last_w = {}
        self.readers = {}
        self.ndma = {e: 0 for e in self.ENG}
        self.n = 0

    def op(self, eng, name, reads=(), writes=(), **kw):
        return self.add(eng, (name, kw), reads, writes, False)

    def dma(self, eng, reads=(), writes=(), **kw):
        return self.add(eng, ("dma_start", kw), reads, writes, True)

    def barrier(self):
        lastc = {}
        lastd = []
        for e in self.ENG:
            cs = [o for o in self.q[e] if not o.dma]
            if cs:
                lastc[e] = cs[-1]
            ds = [o for o in self.q[e] if o.dma]
            lastd += ds[-NDMASEM:]
        for e in self.ENG:
            op = self.add(e, ("nop", {}), (), (), False)
            op.deps = [o for pe, o in lastc.items() if pe != e] + list(lastd)
            for d in op.deps:
                if not d.dma:
                    d.sig = True

    def add(self, eng, fn, reads=(), writes=(), dma=False):
        op = Op()
        op.eng = eng
        op.fn = fn
        op.dma = dma
        op.sig = False
        op.sigcount = 0
        op.gi = self.n
        self.n += 1
        raw = set()
        deps = set()
        for s in reads:
            w = self.last_w.get(s)
            if w is not None:
                raw.add(w)
        for s in writes:
            w = self.last_w.get(s)
            if w is not None:
                deps.add(w)
            for r in self.readers.get(s, ()):
                deps.add(r)
        out = []
        for d in raw | deps:
            if d is op:
                continue
            if d.eng == eng and not d.dma and not dma and d not in raw and eng == "pe":
                continue
            out.append(d)
        op.deps = out
        for d in out:
            if not d.dma:
                d.sig = True
        if dma:
            n = self.ndma[eng]
            self.ndma[eng] = n + 1
            op.dsem = n % NDMASEM
            op.dval = 16 * (n // NDMASEM + 1)
            op.dprev = 16 * (n // NDMASEM)
        for s in reads:
            self.readers.setdefault(s, []).append(op)
        for s in writes:
            self.last_w[s] = op
            self.readers[s] = []
        self.q[eng].append(op)
        return op

    def emit(self, nc, stack, final_wait_ops=()):
        nsem = {}
        for e in self.ENG:
            c = 0
            for op in self.q[e]:
                if op.sig and not op.dma:
                    c += 1
                    op.sigcount = c
            nsem[e] = (c + EPOCH - 1) // EPOCH
        csem = {e: [stack.enter_context(nc.semaphore(f"c_{e}_{i}")) for i in range(nsem[e])] for e in self.ENG}
        dsem = {e: [stack.enter_context(nc.semaphore(f"d_{e}_{i}")) for i in range(NDMASEM)] if self.ndma[e] else [] for e in self.ENG}
        block = stack.enter_context(nc.Block())
        deco = {"pe": block.tensor, "act": block.scalar, "dve": block.vector, "pool": block.gpsimd, "sp": block.sync}
        stats = {}

        def make(e):
            def body(eng):
                waited_c = {p: 0 for p in self.ENG}
                waited_d = {}
                nw = 0
                for op in self.q[e]:
                    need_c = {}
                    need_d = {}
                    for d in op.deps:
                        if d.dma:
                            k = (d.eng, d.dsem)
                            if waited_d.get(k, 0) < d.dval:
                                need_d[k] = max(need_d.get(k, 0), d.dval)
                        else:
                            if waited_c[d.eng] < d.sigcount:
                                need_c[d.eng] = max(need_c.get(d.eng, 0), d.sigcount)
                    if op.dma and op.dprev > 0:
                        k = (e, op.dsem)
                        if waited_d.get(k, 0) < op.dprev:
                            need_d[k] = max(need_d.get(k, 0), op.dprev)
                    for p, cnt in need_c.items():
                        si = (cnt - 1) // EPOCH
                        eng.wait_ge(csem[p][si], (cnt - 1) % EPOCH + 1)
                        waited_c[p] = cnt
                        nw += 1
                    for k, v in need_d.items():
                        eng.wait_ge(dsem[k[0]][k[1]], v)
                        waited_d[k] = v
                        nw += 1
                    ins = getattr(eng, op.fn[0])(**op.fn[1])
                    if op.dma:
                        ins.then_inc(dsem[e][op.dsem], 16)
                    elif op.sig:
                        si = (op.sigcount - 1) // EPOCH
                        ins.then_inc(csem[e][si], 1)
                if e == "sp":
                    for d in final_wait_ops:
                        if d.dma:
                            eng.wait_ge(dsem[d.eng][d.dsem], d.dval)
                        else:
                            si = (d.sigcount - 1) // EPOCH
                            eng.wait_ge(csem[d.eng][si], (d.sigcount - 1) % EPOCH + 1)
                stats[e] = (len(self.q[e]), nw)
            return body

        for e in self.ENG:
            deco[e](make(e))
        return stats


D = 1024
LAM_INIT = 0.8 - 0.6 * math.exp(-0.3 * 0)


class Cfg:
    def __init__(self, T=4096, NH=8, stages="ABC", debug=(), NB=8, NE=16):
        self.T = T
        self.TO = T // 2
        self.NT = T // 128
        self.NTO = self.TO // 128
        self.NCH = T // 512
        self.NCHO = self.TO // 512
        self.NH = NH
        self.stages = stages
        self.NB = NB
        self.NE = NE
        self.debug = debug


def build(cfg):
    T, TO, NT, NTO, NCH, NCHO, NH = cfg.T, cfg.TO, cfg.NT, cfg.NTO, cfg.NCH, cfg.NCHO, cfg.NH
    nc = bass.Bass("TRN2", target_bir_lowering=False)
    din = lambda name, shape, dt=F32: nc.dram_tensor(name, shape, dt, kind="ExternalInput").ap()
    dout = lambda name, shape, dt=F32: nc.dram_tensor(name, shape, dt, kind="ExternalOutput").ap()
    x_d = din("x", [T, D])
    g1_d = din("g1", [1, D])
    cs_d = din("cs", [T, 64])
    gq_d = din("gq", [1, 64])
    gk_d = din("gk", [1, 64])
    lam_d = din("lamv", [4, 64])
    gsub_d = din("gsub", [1, 128])
    wqkv_d = din("wqkv", [NH, 128, 8, 384])
    cp_d = din("cp", [128, 8, 12])
    wlx_d = din("wlx", [8, 128, 8, 128])
    wlg_d = din("wlg", [8, 128, 8, 128])
    wg_d = din("wg", [8, 128, 4, 128])
    NB = cfg.NB
    NE = cfg.NE
    wao_d = din("wao", [8, 128, 8, 128])
    wlo_d = din("wlo", [8, 128, 8, 128])
    wga_d = din("wga", [8, 128, 8, 128])
    wgl_d = din("wgl", [8, 128, 8, 128])
    bg_d = din("bg", [128, 16])
    wout_d = din("wout", [128, 8, 1024])
    g2_d = din("g2", [1, D])
    wr_d = din("wr", [128, 8, 20])
    br_d = din("br", [1, 20])
    weg_d = din("weg", [16, 128, 8, 512])
    weu_d = din("weu", [16, 128, 8, 512])
    wed_d = din("wed", [16, 128, 4, 1024])
    out_d = dout("out", [TO, D])
    dbg = {}
    if "oT" in cfg.debug:
        dbg["oT"] = dout("dbg_oT", [128, NH, TO], BF16)
    if "yT" in cfg.debug:
        dbg["yT"] = dout("dbg_yT", [128, 8, TO], BF16)
    if "KT" in cfg.debug:
        dbg["KT"] = dout("dbg_KT", [128, T], BF16)
        dbg["QT"] = dout("dbg_QT", [128, TO], BF16)
        dbg["V"] = dout("dbg_V", [128, NT, 130], BF16)

    P = Prog()
    fin = []
    with ExitStack() as st:
        sb = lambda name, shape, dt: st.enter_context(nc.sbuf_tensor(name, shape, dt))
        psum = lambda name, shape, dt: st.enter_context(nc.psum_tensor(name, shape, dt))
        hT = sb("hT", [128, 8, T], BF16)
        oT = sb("oT", [128, NH, TO], BF16)

        ident = sb("ident", [128, 128], BF16)
        identf = sb("identf", [128, 128], F32)
        eps = sb("eps", [128, 1], F32)
        neghalf = sb("neghalf", [128, 32], F32)
        gq = sb("gq_s", [128, 64], F32)
        gk = sb("gk_s", [128, 64], F32)
        lamv = sb("lamv_s", [128, 4, 64], F32)
        gsub = sb("gsub_s", [128, 128], F32)
        lam_t = sb("lam_t", [128, 8], F32)
        lam_j = sb("lam_j", [128, 2, 64], F32)
        cp = sb("cp_s", [128, 8, 12], F32)
        hbias = sb("hbias", [128, 8, 4], F32)
        hc = sb("hc", [128, 8, 2], F32)
        lrt = sb("lrt", [128, 8, 2], F32)
        sT = ExitStack()
        TK = sT.enter_context(nc.sbuf_tensor("TK", [128, NT, 4, 32], F32))
        TQ = sT.enter_context(nc.sbuf_tensor("TQ", [128, NTO, 4, 32], F32))
        sA = ExitStack()
        sbA = lambda name, shape, dt: sA.enter_context(nc.sbuf_tensor(name, shape, dt))
        gain1 = sbA("gain1", [128, D], F32)
        cs = sbA("cs_s", [128, NT, 64], F32)

        P.op("pool", "memset", writes=["identf"], ap=identf[:], constant=0.0)
        P.op("pool", "affine_select", reads=["identf"], writes=["identf"], out=identf[:], in_=identf[:], pattern=[[-1, 128]],
                                                 compare_op=ALU.not_equal, fill=1.0, base=0, channel_multiplier=1)
        P.op("pool", "tensor_copy", reads=["identf"], writes=["ident"], out=ident[:], in_=identf[:])
        P.op("pool", "memset", writes=["eps"], ap=eps[:], constant=1e-6)
        P.op("pool", "memset", writes=["neghalf"], ap=neghalf[:], constant=-0.5)
        P.dma("sp", writes=["gain1"], out=gain1[:], in_=g1_d.partition_broadcast(128))
        P.dma("sp", writes=["gq"], out=gq[:], in_=gq_d.partition_broadcast(128))
        P.dma("sp", writes=["gk"], out=gk[:], in_=gk_d.partition_broadcast(128))
        P.dma("sp", writes=["gsub"], out=gsub[:], in_=gsub_d.partition_broadcast(128))
        for i in range(4):
            P.dma("sp", writes=[("lamv", i)], out=lamv[:, i, :], in_=lam_d[i:i + 1, :].partition_broadcast(128))
        P.dma("sp", writes=["cs"], out=cs[:], in_=cs_d.rearrange("(n p) f -> p n f", p=128))
        for (Tt, g, gname, n, tname) in ((TK, gk, "gk", NT, "TK"), (TQ, gq, "gq", NTO, "TQ")):
            for ti, (csoff, goff) in enumerate(((0, 0), (32, 32), (32, 0), (0, 32))):
                P.op("pool", "tensor_tensor", reads=["cs", gname], writes=[tname],
                    out=Tt[:, :, ti, :], in0=cs[:, 0:n, csoff:csoff + 32],
                    in1=g[:, goff:goff + 32].unsqueeze(1).to_broadcast([128, n, 32]), op=ALU.mult)
        for i in range(2):
            P.op("dve", "tensor_tensor", reads=[("lamv", 2 * i), ("lamv", 2 * i + 1)], writes=[("lam_j", i)], out=lam_j[:, i, :], in0=lamv[:, 2 * i, :], in1=lamv[:, 2 * i + 1, :], op=ALU.mult)
        P.op("dve", "tensor_reduce", reads=[("lam_j", 0), ("lam_j", 1)], writes=["lam_t01"], out=lam_t[:, 0:2], in_=lam_j[:], axis=AX.X, op=ALU.add)
        P.op("act", "activation", reads=["lam_t01"], writes=["lam_t23"], out=lam_t[:, 2:4], in_=lam_t[:, 0:2], func=AF.Exp)
        P.op("dve", "tensor_tensor", reads=["lam_t23"], writes=["lam_t4"], out=lam_t[:, 4:5], in0=lam_t[:, 2:3], in1=lam_t[:, 3:4], op=ALU.subtract)
        P.op("dve", "tensor_scalar", reads=["lam_t4"], writes=["neglam"], out=lam_t[:, 5:6], in0=lam_t[:, 4:5], scalar1=-1.0, scalar2=-LAM_INIT,
                                               op0=ALU.mult, op1=ALU.add)
        P.op("dve", "tensor_scalar", reads=["gsub"], writes=["gsub"], out=gsub[:], in0=gsub[:], scalar1=(1.0 - LAM_INIT), scalar2=None, op0=ALU.mult)

        xt = sbA("xt", [128, 8, D], F32)
        junk = sbA("junk", [128, D], BF16)
        ssA = sbA("ssA", [128, 2, 4], F32)
        msA = sbA("msA", [128, 2, 4], F32)
        rsA = sbA("rsA", [128, 2, 4], F32)
        hb = sbA("hb", [128, 2, 4, D], BF16)
        banks = [psum(f"bank{i}", [128, 512], F32) for i in range(8)]
        bk_bf = [b[:].bitcast(BF16) for b in banks]

        nb = 0
        for c in range(NCH):
            cb = c % 2
            for j in range(4):
                tt = c * 4 + j
                P.dma("sp", writes=[("xt", cb * 4 + j)], out=xt[:, cb * 4 + j, :], in_=x_d[tt * 128:(tt + 1) * 128, :])
                P.op("act", "activation", reads=[("xt", cb * 4 + j)], writes=["junk", ("ssA", cb, j)], out=junk[:], in_=xt[:, cb * 4 + j, :], func=AF.Square,
                                                               accum_out=ssA[:, cb, j:j + 1])
            P.op("dve", "tensor_scalar", reads=[("ssA", cb, j) for j in range(4)], writes=[("msA", cb)], out=msA[:, cb, :], in0=ssA[:, cb, :], scalar1=1.0 / D, scalar2=1e-6,
                                                         op0=ALU.mult, op1=ALU.add)
            P.op("pool", "tensor_tensor", reads=[("msA", cb), "neghalf"], writes=[("rsA", cb)], out=rsA[:, cb, :], in0=msA[:, cb, :], in1=neghalf[:, 0:4], op=ALU.pow)
            for j in range(4):
                P.op("dve", "scalar_tensor_tensor", reads=[("xt", cb * 4 + j), ("rsA", cb), "gain1"], writes=[("hb", cb, j)],
                    out=hb[:, cb, j, :], in0=xt[:, cb * 4 + j, :], scalar=rsA[:, cb, j:j + 1], in1=gain1[:],
                    op0=ALU.mult, op1=ALU.mult)
            for kp in range(4):
                bank = nb % 2
                nb += 1
                for kk in range(2):
                    k = kp * 2 + kk
                    for j in range(4):
                        P.op("pe", "transpose", reads=[("hb", cb, j), "ident"], writes=[("bank", bank)],
                            out=bk_bf[bank][:, kk * 512 + j * 128: kk * 512 + (j + 1) * 128],
                            in_=hb[:, cb, j, k * 128:(k + 1) * 128], identity=ident[:])
                P.op("act", "copy", reads=[("bank", bank)], writes=[("hT", c)],
                    out=hT[:, kp * 2:kp * 2 + 2, c * 512:(c + 1) * 512],
                    in_=bk_bf[bank].rearrange("p (a b) -> p a b", a=2))
        P.barrier()
        sA.close()

        sB = ExitStack()
        sbB = lambda name, shape, dt: sB.enter_context(nc.sbuf_tensor(name, shape, dt))
        wh = sbB("wh", [128, 2, 8, 384], BF16)
        KT = sbB("KT", [128, 2, T], BF16)
        QT = sbB("QT", [128, 2, TO], BF16)
        VA = sbB("VA", [128, 2, NT, 130], BF16)
        raw = sbB("raw", [128, 1, 4, 384], F32)
        sqb = sbB("sqb", [128, 4, 256], F32)
        ssB = sbB("ssB", [128, 4, 4], F32)
        msB = sbB("msB", [128, 4, 4], F32)
        rsB = sbB("rsB", [128, 4, 4], F32)
        tA = sbB("tA", [128, 1, 4, 2, 32], F32)
        tB = sbB("tB", [128, 1, 4, 2, 32], F32)
        rot = sbB("rot", [128, 1, 4, 2, 64], F32)
        qkb = sbB("qkb", [128, 2, 4, 128], BF16)
        PT = sbB("PT", [128, 2, 2, 512], BF16)
        rcp = sbB("rcp", [128, 3, 3], F32)
        nrcp = sbB("nrcp", [128, 3, 3], F32)
        ocp = sbB("ocp", [128, 3, 387], F32)
        osq = sqb[:, :, 0:128]
        oss = sbB("oss", [128, 4], F32)
        oms = sbB("oms", [128, 4], F32)
        ors = sbB("ors", [128, 4], F32)
        onb = sbB("onb", [128, 4, 128], BF16)

        PREP = 7
        SB = [(0, 1), (2, 3)]
        OB = [4, 5, 6]
        pS = [None, None]
        for i in range(2):
            pass
        P.op("pool", "memset", writes=[("rcp", 2)], ap=rcp[:], constant=1.0)
        for hb_i in range(2):
            P.op("pool", "memset", writes=[("VA1", hb_i)], ap=VA[:, hb_i, :, 128:130], constant=1.0)

        def slot_ap(j, s):
            idx = j * 2 + s
            b, sl = OB[idx // 3], idx % 3
            return banks[b][:, sl * 129: sl * 129 + 129], b, idx

        def prep_start(h):
            hbi = h % 2
            P.dma("pool", writes=[("wh", hbi)], out=wh[:, hbi], in_=wqkv_d[h])

        def proj_tile(h, c, j):
            hbi = h % 2
            own = c < NCHO
            lo = 0 if own else 128
            rb = 0
            if True:
                if True:
                    tt = c * 4 + j
                    for k in range(8):
                        P.op("pe", "matmul", reads=[("hT", c), ("wh", hbi)], writes=[("bank", PREP)],
                            out=banks[PREP][:, lo:384], lhsT=hT[:, k, tt * 128:(tt + 1) * 128], rhs=wh[:, hbi, k, lo:384],
                            start=(k == 0), stop=(k == 7))
                    P.op("dve", "tensor_copy", reads=[("bank", PREP)], writes=[("raw", rb, j)], out=raw[:, rb, j, lo:384], in_=banks[PREP][:, lo:384])

        def elem(h, c):
            hbi = h % 2
            own = c < NCHO
            lo = 0 if own else 128
            rb = 0
            if True:
                rr = [("raw", rb, j) for j in range(4)]
                ng = 4 if own else 2
                qk = raw[:, rb, :, lo:256]
                P.op("pool", "tensor_tensor", reads=rr, writes=["sqb"], out=sqb[:, :, lo:256], in0=qk, in1=qk, op=ALU.mult)
                gl = lo // 64
                P.op("dve", "tensor_reduce", reads=["sqb"], writes=["ssB"],
                    out=ssB[:, :, gl:4], in_=sqb[:, :, lo:256].rearrange("p t (g d) -> p t g d", d=64), axis=AX.X, op=ALU.add)
                P.op("dve", "tensor_scalar", reads=["ssB"], writes=["msB"], out=msB[:, :, gl:4], in0=ssB[:, :, gl:4], scalar1=1.0 / 64, scalar2=1e-6,
                                                              op0=ALU.mult, op1=ALU.add)
                P.op("pool", "tensor_tensor", reads=["msB", "neghalf"], writes=["rsB"], out=rsB[:, :, gl:4], in0=msB[:, :, gl:4],
                                                               in1=neghalf[:, 0:4 * (4 - gl)].rearrange("p (a b) -> p a b", a=4), op=ALU.pow)
                for qi in ([0, 1] if own else [1]):
                    eng = "dve"
                    xv = raw[:, rb, :, qi * 128:(qi + 1) * 128].rearrange("p t (g d) -> p t g d", d=64)
                    x1 = xv[:, :, :, 0:32]
                    x2 = xv[:, :, :, 32:64]
                    Tt = TQ if qi == 0 else TK
                    tname = "TQ" if qi == 0 else "TK"

                    def tb(ti):
                        return Tt[:, c * 4:c * 4 + 4, ti, :].unsqueeze(2).to_broadcast([128, 4, 2, 32])
                    A, B = tA[:, 0], tB[:, 0]
                    P.op(eng, "tensor_tensor", reads=rr + [tname], writes=["tA"], out=A, in0=x1, in1=tb(0), op=ALU.mult)
                    P.op(eng, "tensor_tensor", reads=rr + [tname], writes=["tB"], out=B, in0=x2, in1=tb(1), op=ALU.mult)
                    P.op(eng, "tensor_tensor", reads=["tA", "tB"], writes=["rot1"], out=rot[:, 0, :, :, 0:32], in0=A, in1=B, op=ALU.subtract)
                    P.op(eng, "tensor_tensor", reads=rr + [tname, "rot1"], writes=["tA"], out=A, in0=x1, in1=tb(2), op=ALU.mult)
                    P.op(eng, "tensor_tensor", reads=rr + [tname, "rot1"], writes=["tB"], out=B, in0=x2, in1=tb(3), op=ALU.mult)
                    P.op(eng, "tensor_tensor", reads=["tA", "tB"], writes=["rot2"], out=rot[:, 0, :, :, 32:64], in0=A, in1=B, op=ALU.add)
                    P.op(eng, "tensor_tensor", reads=["rot1", "rot2", "rsB"], writes=[("qkb", qi)],
                        out=qkb[:, qi].rearrange("p t (g d) -> p t g d", d=64), in0=rot[:, 0],
                        in1=rsB[:, :, 2 * qi:2 * qi + 2].unsqueeze(3).to_broadcast([128, 4, 2, 64]), op=ALU.mult)
                P.op("pool", "tensor_copy", reads=rr, writes=[("VA", hbi, c)], out=VA[:, hbi, c * 4:c * 4 + 4, 0:128], in_=raw[:, rb, :, 256:384])

        def trans(h, c):
            hbi = h % 2
            own = c < NCHO
            if True:
                for qi in ([0, 1] if own else [1]):
                    for j in range(4):
                        P.op("pe", "transpose", reads=[("qkb", qi), "ident"], writes=[("bank", PREP)],
                            out=bk_bf[PREP][:, (qi * 4 + j) * 128:(qi * 4 + j + 1) * 128], in_=qkb[:, qi, j, :], identity=ident[:])
                P.op("dve", "tensor_copy", reads=[("bank", PREP)], writes=[("KT", hbi, c)], out=KT[:, hbi, c * 512:(c + 1) * 512], in_=bk_bf[PREP][:, 512:1024])
                if own:
                    P.op("dve", "tensor_copy", reads=[("bank", PREP)], writes=[("QT", hbi, c)], out=QT[:, hbi, c * 512:(c + 1) * 512], in_=bk_bf[PREP][:, 0:512])

        def attn(h, inject):
            hbi = h % 2
            it = 0
            tot = NCHO * NT
            every = max(1, tot // max(1, len(inject)))
            for qc in range(NCHO):
                def qk_mm(kt, sbi):
                    kc = kt // 4
                    for s in range(2):
                        P.op("pe", "matmul", reads=[("KT", hbi, kc), ("QT", hbi, qc)], writes=[("bank", SB[sbi][s])],
                            out=banks[SB[sbi][s]][:, :], lhsT=KT[s * 64:(s + 1) * 64, hbi, kt * 128:(kt + 1) * 128],
                            rhs=QT[s * 64:(s + 1) * 64, hbi, qc * 512:(qc + 1) * 512], start=True, stop=True)

                def exp_op(kt, sbi, pbi):
                    for s in range(2):
                        P.op("act", "activation", reads=[("bank", SB[sbi][s])], writes=[("PT", pbi, s)], out=PT[:, pbi, s, :], in_=banks[SB[sbi][s]][:, :], func=AF.Exp, scale=0.125)

                def pv_mm(kt, pbi):
                    kc = kt // 4
                    for s in range(2):
                        for j in range(4):
                            oap, b, idx = slot_ap(j, s)
                            first = (kt == 0) and ((j, s) in ((0, 0), (2, 0), (3, 0)))
                            P.op("pe", "matmul", reads=[("PT", pbi, s), ("VA", hbi, kc), ("VA1", hbi)], writes=[("bank", b)],
                                out=oap, lhsT=PT[:, pbi, s, j * 128:(j + 1) * 128], rhs=VA[:, hbi, kt, 0:129],
                                start=first, stop=(kt == NT - 1), skip_group_check=True)

                for kt in range(NT + 1):
                    if kt < NT:
                        qk_mm(kt, (it + kt) % 2)
                        exp_op(kt, (it + kt) % 2, (it + kt) % 2)
                    if kt >= 1:
                        pv_mm(kt - 1, (it + kt - 1) % 2)
                    if kt == min(8, NT - 1) and deferred:
                        deferred.pop(0)()
                    if kt < NT and (it + kt) % every == 0 and inject:
                        inject.pop(0)()
                it += NT
                ob = [("bank", b) for b in OB]
                for bi in range(3):
                    nsl = 3 if bi < 2 else 2
                    P.op("dve", "tensor_copy", reads=[ob[bi]], writes=[("ocp", bi)], out=ocp[:, bi, 0:129 * nsl], in_=banks[OB[bi]][:, 0:129 * nsl])
                for bi in range(3):
                    nsl = 3 if bi < 2 else 2
                    P.op("dve", "reciprocal", reads=[("ocp", bi)], writes=[("rcp", bi)], out=rcp[:, bi, 0:nsl],
                         in_=ocp[:, bi, 128:128 + 129 * (nsl - 1) + 1:129])
                P.op("dve", "tensor_scalar", reads=[("rcp", 0), ("rcp", 1), ("rcp", 2), "neglam"], writes=["nrcp"], out=nrcp[:], in0=rcp[:], scalar1=lam_t[:, 5:6], scalar2=None, op0=ALU.mult)

                def oslot(j, s_):
                    idx = j * 2 + s_
                    return ocp[:, idx // 3, (idx % 3) * 129:(idx % 3) * 129 + 128], idx
                for j in range(4):
                    o1, i1 = oslot(j, 0)
                    o2, i2 = oslot(j, 1)
                    P.op("dve", "tensor_scalar", reads=[("ocp", i1 // 3), ("rcp", i1 // 3)], writes=[("ocp", i1 // 3)],
                        out=o1, in0=o1, scalar1=rcp[:, i1 // 3, i1 % 3:i1 % 3 + 1], scalar2=None, op0=ALU.mult)
                    P.op("dve", "scalar_tensor_tensor", reads=[("ocp", i2 // 3), ("ocp", i1 // 3), "nrcp"], writes=[("ocp", i1 // 3)],
                        out=o1, in0=o2, scalar=nrcp[:, i2 // 3, i2 % 3:i2 % 3 + 1], in1=o1, op0=ALU.mult, op1=ALU.add)
                oc = [("ocp", bi) for bi in range(3)]
                for j in range(4):
                    o1, i1 = oslot(j, 0)
                    P.op("pool", "tensor_tensor", reads=oc, writes=["sqb"], out=osq[:, j, :], in0=o1, in1=o1, op=ALU.mult)
                P.op("dve", "tensor_reduce", reads=["sqb"], writes=["oss"], out=oss[:], in_=osq, axis=AX.X, op=ALU.add)
                P.op("dve", "tensor_scalar", reads=["oss"], writes=["oms"], out=oms[:], in0=oss[:], scalar1=1.0 / 128, scalar2=1e-6, op0=ALU.mult, op1=ALU.add)
                P.op("pool", "tensor_tensor", reads=["oms", "neghalf"], writes=["ors"], out=ors[:], in0=oms[:], in1=neghalf[:, 0:4], op=ALU.pow)
                for j in range(4):
                    o1, i1 = oslot(j, 0)
                    P.op("dve", "scalar_tensor_tensor", reads=oc + ["ors", "gsub"], writes=[("onb", j)],
                        out=onb[:, j, :], in0=o1, scalar=ors[:, j:j + 1], in1=gsub[:], op0=ALU.mult, op1=ALU.mult)

                def fin_o(h=h, qc=qc):
                    for j in range(4):
                        P.op("pe", "transpose", reads=[("onb", j), "ident"], writes=[("bank", PREP)], out=bk_bf[PREP][:, j * 128:(j + 1) * 128], in_=onb[:, j, :], identity=ident[:])
                    P.op("act", "copy", reads=[("bank", PREP)], writes=[("oT", h, qc)], out=oT[:, h, qc * 512:(qc + 1) * 512], in_=bk_bf[PREP][:, 0:512])
                deferred.append(fin_o)

        def prep_steps(h):
            steps = []
            for c in range(NCH):
                for j in range(4):
                    if j < 3:
                        steps.append(lambda c=c, j=j: proj_tile(h, c, j))
                    elif c > 0:
                        steps.append(lambda c=c, j=j: (trans(h, c - 1), proj_tile(h, c, j), elem(h, c)))
                    else:
                        steps.append(lambda c=c, j=j: (proj_tile(h, c, j), elem(h, c)))
            steps.append(lambda: trans(h, NCH - 1))
            return steps

        deferred = []
        prep_start(0)
        for f in prep_steps(0):
            f()
        for h in range(NH):
            inj = []
            if h + 1 < NH:
                prep_start(h + 1)
                inj = prep_steps(h + 1)
            attn(h, inj)
            while inj:
                inj.pop(0)()
            if h == NH - 1:
                while deferred:
                    deferred.pop(0)()
            if "KT" in dbg and h == NH - 1:
                hbi = h % 2
                fin.append(P.dma("sp", reads=[("KT", hbi, c) for c in range(NCH)], out=dbg["KT"], in_=KT[:, hbi, :]))
                fin.append(P.dma("sp", reads=[("QT", hbi, c) for c in range(NCHO)], out=dbg["QT"], in_=QT[:, hbi, :]))
                fin.append(P.dma("sp", reads=[("VA", hbi, c) for c in range(NCH)] + [("VA1", hbi)], out=dbg["V"], in_=VA[:, hbi]))
        if "oT" in dbg:
            fin.append(P.dma("sp", reads=[("oT", h, qc) for h in range(NH) for qc in range(NCHO)], out=dbg["oT"], in_=oT[:]))
        P.barrier()
        sB.close()
        sT.close()

        P.dma("sp", writes=["cp"], out=cp[:], in_=cp_d)
        P.op("dve", "tensor_scalar", reads=["cp"], writes=["hbias"], out=hbias[:], in0=cp[:, :, 6:10], scalar1=0.5, scalar2=None, op0=ALU.mult)
        P.op("act", "activation", reads=["cp"], writes=["lrt"], out=lrt[:], in_=cp[:, :, 10:12], func=AF.Exp, scale=-1.0)
        P.op("act", "activation", reads=["lrt"], writes=["lrt2"], out=lrt[:], in_=lrt[:], func=AF.Ln, bias=1.0)
        P.op("dve", "tensor_scalar", reads=["lrt2"], writes=["hc"], out=hc[:], in0=lrt[:], scalar1=-4.0, scalar2=None, op0=ALU.mult)
        yT = sb("yT", [128, 8, TO], BF16)
        sL = ExitStack()
        sbL = lambda name, shape, dt: sL.enter_context(nc.sbuf_tensor(name, shape, dt))
        wl = sbL("wl_s", [128, 2, 2, 8, 128], BF16)
        wgt = sbL("wgt", [128, 2, 4, 128], BF16)
        xs = sbL("xs", [128, TO + 4], F32)
        xr = sbL("xr", [128, TO], F32)
        xrb = sbL("xrb", [128, TO], BF16)
        Ab = sbL("Ab", [128, TO], F32)
        Ub = sbL("Ub", [128, TO], F32)
        T1 = sbL("T1", [128, TO], F32)
        hA = sbL("hA", [128, TO], F32)
        gg = sbL("gg", [128, TO], F32)
        stB = sbL("stB", [128, 1], F32)
        NCL = TO // 512
        cnt = {"p": 0, "g": 0}
        g2 = T1

        def ck(name):
            return [(name, ch) for ch in range(NCL)]

        def proj_pieces(widx, buf, tok_lo, ntok, dst, dst_col, dkeys, evac_eng):
            pieces = []
            done = 0
            while done < ntok:
                w = min(512, ntok - done)

                def piece(done=done, w=w):
                    bank = cnt["p"] % 4
                    cnt["p"] += 1
                    lo = tok_lo + done
                    for k in range(8):
                        P.op("pe", "matmul", reads=[("hT", lo // 512), ("hT", (lo + w - 1) // 512), ("wl", buf, widx)], writes=[("bank", bank)],
                             out=banks[bank][:, 0:w], lhsT=wl[:, buf, widx, k, :], rhs=hT[:, k, lo:lo + w], start=(k == 0), stop=(k == 7))
                    if evac_eng == "act":
                        P.op("act", "copy", reads=[("bank", bank)], writes=dkeys(dst_col + done, dst_col + done + w), out=dst[:, dst_col + done:dst_col + done + w], in_=banks[bank][:, 0:w])
                    else:
                        P.op("dve", "tensor_copy", reads=[("bank", bank)], writes=dkeys(dst_col + done, dst_col + done + w), out=dst[:, dst_col + done:dst_col + done + w], in_=banks[bank][:, 0:w])
                pieces.append(piece)
                done += w
            return pieces

        def xskeys(c0, c1):
            return [("xs", i) for i in range(c0 // 512, (c1 - 1) // 512 + 1)]

        def ggkeys(c0, c1):
            return [("gg", i) for i in range(c0 // 512, (c1 - 1) // 512 + 1)]

        def conv_chunk(n, ch):
            lo = ch * 512
            xk = xskeys(lo, lo + 516)
            P.op("dve", "tensor_scalar", reads=xk + ["cp"], writes=[("xr", ch)], out=xr[:, lo:lo + 512], in0=xs[:, lo:lo + 512], scalar1=cp[:, n, 0:1], scalar2=cp[:, n, 5:6],
                 op0=ALU.mult, op1=ALU.add)
            for j in range(1, 5):
                P.op("dve", "scalar_tensor_tensor", reads=xk + ["cp", ("xr", ch)], writes=[("xr", ch)], out=xr[:, lo:lo + 512], in0=xs[:, lo + j:lo + j + 512], scalar=cp[:, n, j:j + 1],
                     in1=xr[:, lo:lo + 512], op0=ALU.mult, op1=ALU.add)
            P.op("pool", "tensor_copy", reads=[("xr", ch)], writes=[("xrb", ch)], out=xrb[:, lo:lo + 512], in_=xr[:, lo:lo + 512])

        def front_end(n, phase):
            buf = n % 2
            if phase == "O":
                first = [lambda: P.op("pool", "memset", reads=[], writes=xskeys(TO + 2, TO + 4), ap=xs[:, TO + 2:TO + 4], constant=0.0)]
                pcs = proj_pieces(0, buf, TO - 2, TO + 2, xs, 0, xskeys, "dve")
            else:
                first = [lambda: P.op("pool", "memset", reads=[], writes=xskeys(0, 2), ap=xs[:, 0:2], constant=0.0)]
                pcs = proj_pieces(0, buf, 0, TO + 2, xs, 2, xskeys, "dve")
            pcs = first + pcs
            state = {"i": 0}

            def hook(ch):
                need = len(pcs) if ch == NCL - 1 else min(len(pcs), ch + 3)
                while state["i"] < need:
                    pcs[state["i"]]()
                    state["i"] += 1
                conv_chunk(n, ch)
            return hook

        def lru_dir(n, buf, d, reverse, init, init_reads, out, okey, hook=None):
            def gates(ch):
                sl = slice(ch * 512, (ch + 1) * 512)
                bx = 4 + cnt["g"] % 4
                by = 4 + (cnt["g"] + 1) % 4
                cnt["g"] += 2
                P.op("pe", "matmul", reads=[("xrb", ch), ("wgt", buf)], writes=[("bank", bx)], out=banks[bx][:, :], lhsT=wgt[:, buf, 2 * d, :], rhs=xrb[:, sl], start=True, stop=True)
                P.op("pe", "matmul", reads=[("xrb", ch), ("wgt", buf)], writes=[("bank", by)], out=banks[by][:, :], lhsT=wgt[:, buf, 2 * d + 1, :], rhs=xrb[:, sl], start=True, stop=True)
                return bx, by
            nxt = gates(0)
            for ch in range(NCL):
                sl = slice(ch * 512, (ch + 1) * 512)
                bx, by = nxt
                P.op("act", "activation", reads=[("bank", bx), "hbias"], writes=[("T1", ch)], out=T1[:, sl], in_=banks[bx][:, :], func=AF.Tanh, scale=0.5, bias=hbias[:, n, d:d + 1])
                P.op("act", "activation", reads=[("bank", by), "hbias"], writes=[("U", ch)], out=Ub[:, sl], in_=banks[by][:, :], func=AF.Tanh, scale=0.5, bias=hbias[:, n, 2 + d:3 + d])
                P.op("act", "activation", reads=[("T1", ch), "hc"], writes=[("A", ch)], out=Ab[:, sl], in_=T1[:, sl], func=AF.Exp, scale=hc[:, n, d:d + 1], bias=hc[:, n, d:d + 1])
                if ch + 1 < NCL:
                    nxt = gates(ch + 1)
                P.op("pool", "tensor_scalar", reads=[("A", ch)], writes=[("A", ch)], out=Ab[:, sl], in0=Ab[:, sl], scalar1=1.0, scalar2=-3.0e38, op0=ALU.min, op1=ALU.max)
                P.op("pool", "tensor_tensor", reads=[("A", ch)], writes=[("T1", ch)], out=T1[:, sl], in0=Ab[:, sl], in1=Ab[:, sl], op=ALU.mult)
                P.op("dve", "scalar_tensor_tensor", reads=[("U", ch), ("xr", ch)], writes=[("U", ch)], out=Ub[:, sl], in0=Ub[:, sl], scalar=1.0, in1=xr[:, sl], op0=ALU.add, op1=ALU.mult)
                if hook is not None:
                    hook(ch)
            P.op("act", "activation", reads=ck("T1"), writes=ck("T1"), out=T1[:], in_=T1[:], func=AF.Sqrt, scale=-1.0, bias=1.0)
            P.op("dve", "scalar_tensor_tensor", reads=ck("U") + ck("T1"), writes=ck("U"), out=Ub[:], in0=Ub[:], scalar=0.5, in1=T1[:], op0=ALU.mult, op1=ALU.mult)
            if reverse:
                P.op("dve", "tensor_tensor_scan", reads=ck("A") + ck("U") + init_reads, writes=okey, out=out[:, ::-1], data0=Ab[:, ::-1], data1=Ub[:, ::-1], initial=init,
                     op0=ALU.mult, op1=ALU.add)
            else:
                P.op("dve", "tensor_tensor_scan", reads=ck("A") + ck("U") + init_reads, writes=okey, out=out[:], data0=Ab[:], data1=Ub[:], initial=init, op0=ALU.mult, op1=ALU.add)

        def l_weights(n):
            buf = n % 2
            P.dma("pool", writes=[("wl", buf, 0)], out=wl[:, buf, 0], in_=wlx_d[n])
            P.dma("pool", writes=[("wl", buf, 1)], out=wl[:, buf, 1], in_=wlg_d[n])
            P.dma("pool", writes=[("wgt", buf)], out=wgt[:, buf], in_=wg_d[n])

        def l_mid(n):
            buf = n % 2
            if n + 1 < NB:
                l_weights(n + 1)
            lru_dir(n, buf, 1, True, 0.0, [], Ab, ck("A"), hook=front_end(n, "W"))
            P.op("dve", "tensor_copy", reads=ck("A"), writes=["stB"], out=stB[:], in_=Ab[:, 0:1])
            gpcs = proj_pieces(1, buf, 0, TO, gg, 0, ggkeys, "dve")
            lru_dir(n, buf, 0, False, 0.0, [], hA, ["hA"], hook=lambda ch: gpcs[ch]() if ch < len(gpcs) else None)
            for f in gpcs[NCL:]:
                f()
            nxt = front_end(n + 1, "O") if n + 1 < NB else None
            lru_dir(n, buf, 1, True, stB[:, 0:1], ["stB"], Ab, ck("A"), hook=nxt)

        def l_tail(n):
            P.op("pool", "tensor_tensor", reads=["hA"] + ck("A"), writes=["hA"], out=hA[:], in0=hA[:], in1=Ab[:], op=ALU.add)
            P.op("pool", "tensor_tensor", reads=ck("gg"), writes=ck("T1"), out=g2[:], in0=gg[:], in1=gg[:], op=ALU.mult)
            P.op("pool", "tensor_scalar", reads=ck("T1"), writes=ck("T1"), out=g2[:], in0=g2[:], scalar1=0.044715, scalar2=1.0, op0=ALU.mult, op1=ALU.add)
            P.op("dve", "tensor_tensor", reads=ck("T1") + ck("gg"), writes=ck("T1"), out=g2[:], in0=g2[:], in1=gg[:], op=ALU.mult)
            P.op("act", "activation", reads=ck("T1"), writes=ck("T1"), out=g2[:], in_=g2[:], func=AF.Tanh, scale=0.7978845608028654)
            P.op("dve", "scalar_tensor_tensor", reads=ck("T1") + ck("gg"), writes=ck("T1"), out=g2[:], in0=g2[:], scalar=1.0, in1=gg[:], op0=ALU.add, op1=ALU.mult)
            P.op("dve", "scalar_tensor_tensor", reads=["hA"] + ck("T1"), writes=[("yT", n)], out=yT[:, n, :], in0=hA[:], scalar=0.5, in1=g2[:], op0=ALU.mult, op1=ALU.mult)

        l_weights(0)
        h0 = front_end(0, "O")
        for ch in range(NCL):
            h0(ch)
        for n in range(NB):
            l_mid(n)
            l_tail(n)
        if "yT" in dbg:
            fin.append(P.dma("sp", reads=[("yT", n) for n in range(NB)], out=dbg["yT"], in_=yT[:]))
        P.barrier()
        sL.close()

        sM = ExitStack()
        sbM = lambda name, shape, dt: sM.enter_context(nc.sbuf_tensor(name, shape, dt))
        mT = sbM("mT", [128, 8, TO], BF16)
        cw = sbM("cw", [128, NTO, 16], F32)
        bg = sbM("bg_s", [128, 16], F32)
        gain2 = sbM("gain2", [128, D], F32)
        wr32 = sbM("wr32", [128, 8, 20], F32)
        wrb = sbM("wrb", [128, 8, 20], BF16)
        brb = sbM("brb", [128, 20], F32)
        sM1 = ExitStack()
        sbM1 = lambda name, shape, dt: sM1.enter_context(nc.sbuf_tensor(name, shape, dt))
        wm = sbM1("wm_s", [128, 2, 4, 8, 128], BF16)
        gsb = sbM1("gsb", [128, 2, 2, 512], F32)
        P.dma("sp", writes=["bg"], out=bg[:], in_=bg_d)
        P.dma("sp", writes=["gain2"], out=gain2[:], in_=g2_d.partition_broadcast(128))
        P.dma("sp", writes=["wr32"], out=wr32[:], in_=wr_d)
        P.dma("sp", writes=["brb"], out=brb[:], in_=br_d.partition_broadcast(128))
        P.op("pool", "tensor_copy", reads=["wr32"], writes=["wrb"], out=wrb[:], in_=wr32[:])
        it = 0
        for ec in range(8):
            buf = ec % 2
            for wi_, wd_ in enumerate((wao_d, wlo_d, wga_d, wgl_d)):
                P.dma("pool", writes=[("wm", buf, wi_)], out=wm[:, buf, wi_], in_=wd_[ec])
            for tc in range(NCHO):
                st_ = it % 2
                it += 1
                bb = [4 * st_ + i for i in range(4)]
                tsl = slice(tc * 512, (tc + 1) * 512)
                srcs = [(oT, [("oT", h, tc) for h in range(NH)]), (yT, [("yT", n) for n in range(NB)]), (hT, [("hT", tc)]), (hT, [("hT", tc)])]
                for wi_ in range(4):
                    src, rk = srcs[wi_]
                    for k in range(8):
                        P.op("pe", "matmul", reads=rk + [("wm", buf, wi_)], writes=[("bank", bb[wi_])], out=banks[bb[wi_]][:, :],
                             lhsT=wm[:, buf, wi_, k, :], rhs=src[:, k, tsl], start=(k == 0), stop=(k == 7))
                P.op("act", "activation", reads=[("bank", bb[2]), "bg"], writes=[("gsb", st_, 0)], out=gsb[:, st_, 0, :], in_=banks[bb[2]][:, :], func=AF.Sigmoid, bias=bg[:, ec:ec + 1])
                P.op("act", "activation", reads=[("bank", bb[3]), "bg"], writes=[("gsb", st_, 1)], out=gsb[:, st_, 1, :], in_=banks[bb[3]][:, :], func=AF.Sigmoid, bias=bg[:, 8 + ec:9 + ec])
                P.op("dve", "tensor_tensor", reads=[("gsb", st_, 0), ("bank", bb[0])], writes=[("gsb", st_, 0)], out=gsb[:, st_, 0, :], in0=gsb[:, st_, 0, :], in1=banks[bb[0]][:, :], op=ALU.mult)
                P.op("dve", "tensor_tensor", reads=[("gsb", st_, 1), ("bank", bb[1])], writes=[("gsb", st_, 1)], out=gsb[:, st_, 1, :], in0=gsb[:, st_, 1, :], in1=banks[bb[1]][:, :], op=ALU.mult)
                P.op("dve", "tensor_tensor", reads=[("gsb", st_, 0), ("gsb", st_, 1)], writes=[("mT", ec, tc)], out=mT[:, ec, tsl], in0=gsb[:, st_, 0, :], in1=gsb[:, st_, 1, :], op=ALU.add)
        P.barrier()
        sM1.close()
        x1 = hT[:].rearrange("p a b -> p (a b)").bitcast(F32).rearrange("p (a b) -> p a b", b=D)
        h2T = oT
        sM2 = ExitStack()
        sbM2 = lambda name, shape, dt: sM2.enter_context(nc.sbuf_tensor(name, shape, dt))
        wo = sbM2("wo_s", [128, 8, D], BF16)
        xt2 = sbM2("xt2", [128, 2, D], F32)
        junk2 = sbM2("junk2", [128, D], BF16)
        ss2 = sbM2("ss2", [128, 4], F32)
        ms2 = sbM2("ms2", [128, 4], F32)
        rs2 = sbM2("rs2", [128, 4], F32)
        h2b = sbM2("h2b", [128, 4, D], BF16)
        lg = sbM2("lg", [128, 20], F32)
        rt = sbM2("rt", [128, 352], F32)
        P.dma("pool", writes=["wo"], out=wo[:], in_=wout_d)
        for c in range(NCHO):
            for j in range(4):
                tt = c * 4 + j
                xb = tt % 2
                P.dma("sp", writes=[("xt2", xb)], out=xt2[:, xb, :], in_=x_d[tt * 128:(tt + 1) * 128, :])
                for half in range(2):
                    bank = (tt * 2 + half) % 4
                    for ec in range(8):
                        P.op("pe", "matmul", reads=[("mT", ec, c), "wo"], writes=[("bank", bank)], out=banks[bank][:, :],
                             lhsT=mT[:, ec, tt * 128:(tt + 1) * 128], rhs=wo[:, ec, half * 512:(half + 1) * 512], start=(ec == 0), stop=(ec == 7))
                    P.op("dve", "tensor_tensor", reads=[("bank", bank), ("xt2", xb)], writes=[("x1", tt)], out=x1[:, tt, half * 512:(half + 1) * 512],
                         in0=xt2[:, xb, half * 512:(half + 1) * 512], in1=banks[bank][:, :], op=ALU.add)
                P.op("act", "activation", reads=[("x1", tt)], writes=["junk2", ("ss2", j)], out=junk2[:], in_=x1[:, tt, :], func=AF.Square, accum_out=ss2[:, j:j + 1])
            P.op("dve", "tensor_scalar", reads=[("ss2", j) for j in range(4)], writes=["ms2"], out=ms2[:], in0=ss2[:], scalar1=1.0 / D, scalar2=1e-6, op0=ALU.mult, op1=ALU.add)
            P.op("pool", "tensor_tensor", reads=["ms2", "neghalf"], writes=["rs2"], out=rs2[:], in0=ms2[:], in1=neghalf[:, 0:4], op=ALU.pow)
            for j in range(4):
                tt = c * 4 + j
                P.op("dve", "scalar_tensor_tensor", reads=[("x1", tt), "rs2", "gain2"], writes=[("h2b", j)], out=h2b[:, j, :], in0=x1[:, tt, :], scalar=rs2[:, j:j + 1], in1=gain2[:],
                     op0=ALU.mult, op1=ALU.mult)
            for kp in range(4):
                bank = 4 + kp % 2
                for kk in range(2):
                    k = kp * 2 + kk
                    for j in range(4):
                        P.op("pe", "transpose", reads=[("h2b", j), "ident"], writes=[("bank", bank)],
                             out=bk_bf[bank][:, kk * 512 + j * 128: kk * 512 + (j + 1) * 128], in_=h2b[:, j, k * 128:(k + 1) * 128], identity=ident[:])
                P.op("act", "copy", reads=[("bank", bank)], writes=[("h2T", c)], out=h2T[:, kp * 2:kp * 2 + 2, c * 512:(c + 1) * 512],
                     in_=bk_bf[bank].rearrange("p (a b) -> p a b", a=2))
            for j in range(4):
                tt = c * 4 + j
                for k in range(8):
                    P.op("pe", "matmul", reads=[("h2T", c), "wrb"], writes=[("bank", 6)], out=banks[6][:, j * 20:(j + 1) * 20], lhsT=h2T[:, k, tt * 128:(tt + 1) * 128], rhs=wrb[:, k, :],
                         start=(k == 0), stop=(k == 7), skip_group_check=True)
            lg4 = rt[:, 0:80].rearrange("p (t f) -> p t f", f=20)
            P.op("dve", "tensor_tensor", reads=[("bank", 6), "brb"], writes=["lg4"], out=lg4, in0=banks[6][:, 0:80].rearrange("p (t f) -> p t f", f=20),
                 in1=brb[:].unsqueeze(1).to_broadcast([128, 4, 20]), op=ALU.add)
            glog = lg4[:, :, 0:4]
            elog = lg4[:, :, 4:20].rearrange("p t (g e) -> p t g e", e=4)
            V = lambda o, n: rt[:, o:o + n]
            V3 = lambda o: rt[:, o:o + 16].rearrange("p (t e) -> p t e", e=4)
            bc3 = lambda ap4: ap4.unsqueeze(2).to_broadcast([128, 4, 4])
            gmax, gsh, gexp, gsum, gw, goh = V(80, 4), V3(84), V3(100), V(116, 4), V(120, 4), V3(124)
            tmp4 = rt[:, 140:204].rearrange("p (t g e) -> p t g e", g=4, e=4)
            sel, m1, oh1, sel2, m2, oh2 = V3(204), V(220, 4), V3(224), V3(240), V(256, 4), V3(260)
            dd, e2, den, w1, w2, t1, t2, cwe = V(276, 4), V(280, 4), V(284, 4), V(288, 4), V(292, 4), V3(296), V3(312), V3(328)
            P.op("dve", "tensor_reduce", reads=["lg4"], writes=["gmax"], out=gmax, in_=glog, axis=AX.X, op=ALU.max)
            P.op("dve", "tensor_tensor", reads=["lg4", "gmax"], writes=["goh"], out=goh, in0=glog, in1=bc3(gmax), op=ALU.is_equal)
            P.op("dve", "tensor_tensor", reads=["lg4", "gmax"], writes=["gsh"], out=gsh, in0=glog, in1=bc3(gmax), op=ALU.subtract)
            P.op("act", "activation", reads=["gsh"], writes=["gexp"], out=gexp, in_=gsh, func=AF.Exp)
            P.op("dve", "tensor_reduce", reads=["gexp"], writes=["gsum"], out=gsum, in_=gexp, axis=AX.X, op=ALU.add)
            P.op("dve", "reciprocal", reads=["gsum"], writes=["gw"], out=gw, in_=gsum)
            P.op("dve", "tensor_tensor", reads=["lg4", "goh"], writes=["tmp4"], out=tmp4, in0=elog, in1=goh.unsqueeze(3).to_broadcast([128, 4, 4, 4]), op=ALU.mult)
            P.op("dve", "tensor_reduce", reads=["tmp4"], writes=["sel"], out=sel, in_=tmp4.rearrange("p t g e -> p t e g"), axis=AX.X, op=ALU.add)
            P.op("dve", "tensor_reduce", reads=["sel"], writes=["m1"], out=m1, in_=sel, axis=AX.X, op=ALU.max)
            P.op("dve", "tensor_tensor", reads=["sel", "m1"], writes=["oh1"], out=oh1, in0=sel, in1=bc3(m1), op=ALU.is_equal)
            P.op("dve", "scalar_tensor_tensor", reads=["oh1", "sel"], writes=["sel2"], out=sel2, in0=oh1, scalar=-1e30, in1=sel, op0=ALU.mult, op1=ALU.add)
            P.op("dve", "tensor_reduce", reads=["sel2"], writes=["m2"], out=m2, in_=sel2, axis=AX.X, op=ALU.max)
            P.op("dve", "tensor_tensor", reads=["sel2", "m2"], writes=["oh2"], out=oh2, in0=sel2, in1=bc3(m2), op=ALU.is_equal)
            P.op("dve", "tensor_tensor", reads=["m2", "m1"], writes=["dd"], out=dd, in0=m2, in1=m1, op=ALU.subtract)
            P.op("act", "activation", reads=["dd"], writes=["e2"], out=e2, in_=dd, func=AF.Exp)
            P.op("dve", "tensor_scalar", reads=["e2"], writes=["den"], out=den, in0=e2, scalar1=1.0, scalar2=None, op0=ALU.add)
            P.op("dve", "reciprocal", reads=["den"], writes=["w1a"], out=w1, in_=den)
            P.op("dve", "tensor_tensor", reads=["w1a", "gw"], writes=["w1"], out=w1, in0=w1, in1=gw, op=ALU.mult)
            P.op("dve", "tensor_tensor", reads=["w1", "e2"], writes=["w2"], out=w2, in0=w1, in1=e2, op=ALU.mult)
            P.op("dve", "tensor_tensor", reads=["oh1", "w1"], writes=["t1"], out=t1, in0=oh1, in1=bc3(w1), op=ALU.mult)
            P.op("dve", "tensor_tensor", reads=["oh2", "w2"], writes=["t2"], out=t2, in0=oh2, in1=bc3(w2), op=ALU.mult)
            P.op("dve", "tensor_tensor", reads=["t1", "t2"], writes=["cwe"], out=cwe, in0=t1, in1=t2, op=ALU.add)
            P.op("dve", "tensor_tensor", reads=["goh", "cwe"], writes=[("cw", c * 4 + j) for j in range(4)],
                 out=cw[:, c * 4:c * 4 + 4, :].rearrange("p t (g e) -> p t g e", e=4),
                 in0=goh.unsqueeze(3).to_broadcast([128, 4, 4, 4]), in1=cwe.unsqueeze(2).to_broadcast([128, 4, 4, 4]), op=ALU.mult)
        P.barrier()
        sM2.close()

        sE = ExitStack()
        sbE = lambda name, shape, dt: sE.enter_context(nc.sbuf_tensor(name, shape, dt))
        if TO >= 2048:
            yflat = yT[:].rearrange("p a b -> p (a b)")
            mflat = mT[:].rearrange("p a b -> p (a b)")
            weg = yflat[:, 0:8192].rearrange("p (u k f) -> p u k f", u=2, k=8)
            weu = yflat[:, 8192:16384].rearrange("p (u k f) -> p u k f", u=2, k=8)
            wed = mflat[:, 0:8192].rearrange("p (u f d) -> p u f d", u=2, f=4)
            hid = mflat[:, 8192:12288].rearrange("p (u f t) -> p u f t", u=2, f=4)
        else:
            weg = sbE("weg_s", [128, 2, 8, 512], BF16)
            weu = sbE("weu_s", [128, 2, 8, 512], BF16)
            wed = sbE("wed_s", [128, 2, 4, D], BF16)
            hid = sbE("hid", [128, 2, 4, 512], BF16)
        sg = sbE("sg", [128, 2, 512], F32)
        itE = 0
        for e_ in range(NE):
            buf = e_ % 2
            P.dma("pool", writes=[("weg", buf)], out=weg[:, buf], in_=weg_d[e_])
            P.dma("pool", writes=[("weu", buf)], out=weu[:, buf], in_=weu_d[e_])
            P.dma("pool", writes=[("wed", buf)], out=wed[:, buf], in_=wed_d[e_])
            for tc in range(NCHO):
                hb_ = itE % 2
                itE += 1
                tsl = slice(tc * 512, (tc + 1) * 512)
                for fc in range(4):
                    gb = (fc % 2) * 2
                    ub = gb + 1
                    for k in range(8):
                        P.op("pe", "matmul", reads=[("h2T", tc), ("weg", buf)], writes=[("bank", gb)], out=banks[gb][:, :],
                             lhsT=weg[:, buf, k, fc * 128:(fc + 1) * 128], rhs=h2T[:, k, tsl], start=(k == 0), stop=(k == 7))
                    for k in range(8):
                        P.op("pe", "matmul", reads=[("h2T", tc), ("weu", buf)], writes=[("bank", ub)], out=banks[ub][:, :],
                             lhsT=weu[:, buf, k, fc * 128:(fc + 1) * 128], rhs=h2T[:, k, tsl], start=(k == 0), stop=(k == 7))
                    P.op("act", "activation", reads=[("bank", gb)], writes=[("sg", fc % 2)], out=sg[:, fc % 2, :], in_=banks[gb][:, :], func=AF.Silu)
                    P.op("dve", "tensor_tensor", reads=[("sg", fc % 2), ("bank", ub)], writes=[("hid", hb_, fc)], out=hid[:, hb_, fc, :], in0=sg[:, fc % 2, :], in1=banks[ub][:, :], op=ALU.mult)
                for tj in range(4):
                    tt = tc * 4 + tj
                    for half in range(2):
                        db = 4 + (tj * 2 + half) % 4
                        for fc in range(4):
                            P.op("pe", "matmul", reads=[("hid", hb_, fc), ("wed", buf)], writes=[("bank", db)], out=banks[db][:, :],
                                 lhsT=hid[:, hb_, fc, tj * 128:(tj + 1) * 128], rhs=wed[:, buf, fc, half * 512:(half + 1) * 512], start=(fc == 0), stop=(fc == 3))
                        P.op("dve", "scalar_tensor_tensor", reads=[("bank", db), ("cw", tt), ("x1", tt)], writes=[("x1", tt)], out=x1[:, tt, half * 512:(half + 1) * 512],
                             in0=banks[db][:, :], scalar=cw[:, tt, e_:e_ + 1], in1=x1[:, tt, half * 512:(half + 1) * 512], op0=ALU.mult, op1=ALU.add)
                    if e_ == NE - 1:
                        fin.append(P.dma("sp", reads=[("x1", tt)], out=out_d[tt * 128:(tt + 1) * 128, :], in_=x1[:, tt, :]))
        sE.close()
        sM.close()

        stats = P.emit(nc, st, final_wait_ops=fin)
        print("stats", stats)
    return nc


_NC_CACHE = {}


def _blk(W):
    return np.ascontiguousarray(W.reshape(8, 128, 8, 128).transpose(2, 1, 0, 3))


def _pc(v):
    return v.reshape(8, 128).T


def make_in_maps(inp, T):
    f32 = np.float32
    B = inp["x"].shape[0]
    w_in = np.asarray(inp["w_in"][0], f32)
    inv = 10000.0 ** (-np.arange(0, 64, 2, dtype=np.float64) / 64.0)
    pos = np.arange(T, dtype=np.float64)
    ang = pos[:, None] * inv[None, :]
    cs_f = np.concatenate([np.cos(ang), np.sin(ang)], 1).astype(f32)
    wqkv = np.zeros((8, 128, 8, 384), f32)
    for i in range(3):
        Wp = w_in[:, i * 1024:(i + 1) * 1024].reshape(8, 128, 8, 128)
        wqkv[:, :, :, i * 128:(i + 1) * 128] = Wp.transpose(2, 1, 0, 3)
    shared = {
        "g1": np.asarray(inp["norm1_gain"][0][None, :], f32),
        "gq": np.asarray(inp["q_norm_gain"][0][None, :], f32),
        "gk": np.asarray(inp["k_norm_gain"][0][None, :], f32),
        "lamv": np.stack([inp["lambda_q1"][0], inp["lambda_k1"][0], inp["lambda_q2"][0], inp["lambda_k2"][0]]).astype(f32),
        "gsub": np.asarray(inp["attn_subln_gain"][0][None, :], f32),
        "wqkv": wqkv,
        "wlx": _blk(w_in[:, 3072:4096]), "wlg": _blk(w_in[:, 4096:5120]),
        "wga": _blk(w_in[:, 5120:6144]), "wgl": _blk(w_in[:, 6144:7168]),
        "wao": _blk(np.asarray(inp["w_attn_o"][0], f32)), "wlo": _blk(np.asarray(inp["w_lru_o"][0], f32)),
        "bg": np.ascontiguousarray(np.asarray(inp["b_gates"][0], f32).reshape(16, 128).T),
        "wout": np.ascontiguousarray(np.asarray(inp["w_out"][0], f32).reshape(8, 128, 1024).transpose(1, 0, 2)),
        "g2": np.asarray(inp["norm2_gain"][0][None, :], f32),
        "wr": np.ascontiguousarray(np.concatenate([inp["w_group_router"][0], inp["w_expert_router"][0]], 1).astype(f32).reshape(8, 128, 20).transpose(1, 0, 2)),
        "br": np.concatenate([inp["b_group_router"][0], inp["b_expert_router"][0]]).astype(f32)[None, :],
        "weg": np.ascontiguousarray(np.asarray(inp["w_expert_gate"][0], f32).reshape(16, 8, 128, 512).transpose(0, 2, 1, 3)),
        "weu": np.ascontiguousarray(np.asarray(inp["w_expert_up"][0], f32).reshape(16, 8, 128, 512).transpose(0, 2, 1, 3)),
        "wed": np.ascontiguousarray(np.asarray(inp["w_expert_down"][0], f32).reshape(16, 4, 128, 1024).transpose(0, 2, 1, 3)),
    }
    conv_w = np.asarray(inp["conv_w"][0], f32)
    conv_b = np.asarray(inp["conv_b"][0], f32)
    wa = np.asarray(inp["lru_wa"][0], f32); wi = np.asarray(inp["lru_wi"][0], f32)
    ba = np.asarray(inp["lru_ba"][0], f32); bi = np.asarray(inp["lru_bi"][0], f32)
    lam = np.asarray(inp["lru_lambda"][0], f32)
    per_half = []
    for half in range(2):
        dA, dB = (0, 1) if half == 0 else (1, 0)
        cp = np.zeros((128, 8, 12), f32)
        taps = [None, conv_w[0], conv_w[1], conv_w[2], conv_w[3]] if half == 0 else [conv_w[3], conv_w[2], conv_w[1], conv_w[0], None]
        for j, tp in enumerate(taps):
            if tp is not None:
                cp[:, :, j] = _pc(tp)
        cp[:, :, 5] = _pc(conv_b)
        cp[:, :, 6] = _pc(ba[dA]); cp[:, :, 7] = _pc(ba[dB]); cp[:, :, 8] = _pc(bi[dA]); cp[:, :, 9] = _pc(bi[dB])
        cp[:, :, 10] = _pc(lam[dA]); cp[:, :, 11] = _pc(lam[dB])
        wg = np.zeros((8, 128, 4, 128), f32)
        for n in range(8):
            wg[n, :, 0] = wa[dA, n]; wg[n, :, 1] = wi[dA, n]; wg[n, :, 2] = wa[dB, n]; wg[n, :, 3] = wi[dB, n]
        cs = cs_f if half == 0 else np.ascontiguousarray(cs_f[::-1])
        per_half.append({"cp": cp, "wg": wg, "cs": cs})
    maps = []
    for c in range(2 * B):
        b, half = c // 2, c % 2
        xb = np.asarray(inp["x"][b], f32)
        m = dict(shared)
        m.update(per_half[half])
        m["x"] = xb if half == 0 else np.ascontiguousarray(xb[::-1])
        maps.append(m)
    return maps


def assemble(results, B, T):
    TO = T // 2
    out = np.zeros((B, T, 1024), np.float32)
    for c in range(2 * B):
        b, half = c // 2, c % 2
        r = np.asarray(results[c]["out"], np.float32)
        if half == 0:
            out[b, :TO] = r
        else:
            out[b, TO:] = r[::-1]
    return out


def kernel(**inputs):
    T = inputs["x"].shape[1]
    B = inputs["x"].shape[0]
    key = (T,)
    if key not in _NC_CACHE:
        _NC_CACHE[key] = build(Cfg(T=T))
    nc = _NC_CACHE[key]
    maps = make_in_maps(inputs, T)
    res = run_bass_kernel_spmd(nc, maps, core_ids=list(range(2 * B)))
    return assemble(res.results, B, T)
```

```python
import math
import numpy as np
from contextlib import ExitStack
import concourse.bass as bass
import concourse.mybir as mybir
from concourse.bass_utils import run_bass_kernel_spmd


F32 = mybir.dt.float32
BF16 = mybir.dt.bfloat16
AF = mybir.ActivationFunctionType
ALU = mybir.AluOpType
AX = mybir.AxisListType

EPOCH = 16000
NDMASEM = 8


class Op:
    __slots__ = ("eng", "fn", "deps", "sig", "sigcount", "dma", "dsem", "dval", "dprev", "gi")


class Prog:
    ENG = ["pe", "act", "dve", "pool", "sp"]

    def __init__(self):
        self.q = {e: [] for e in self.ENG}
        self.last_w = {}
        self.readers = {}
        self.ndma = {e: 0 for e in self.ENG}
        self.n = 0

    def op(self, eng, name, reads=(), writes=(), **kw):
        return self.add(eng, (name, kw), reads, writes, False)

    def dma(self, eng, reads=(), writes=(), **kw):
        return self.add(eng, ("dma_start", kw), reads, writes, True)

    def barrier(self):
        lastc = {}
        lastd = []
        for e in self.ENG:
            cs = [o for o in self.q[e] if not o.dma]
            if cs:
                lastc[e] = cs[-1]
            ds = [o for o in self.q[e] if o.dma]
            lastd += ds[-NDMASEM:]
        for e in self.ENG:
            op = self.add(e, ("nop", {}), (), (), False)
            op.deps = [o for pe, o in lastc.items() if pe != e] + list(lastd)
            for d in op.deps:
                if not d.dma:
                    d.sig = True

    def add(self, eng, fn, reads=(), writes=(), dma=False):
        op = Op()
        op.eng = eng
        op.fn = fn
        op.dma = dma
        op.sig = False
        op.sigcount = 0
        op.gi = self.n
        self.n += 1
        raw = set()
        deps = set()
        for s in reads:
            w = self.last_w.get(s)
            if w is not None:
                raw.add(w)
        for s in writes:
            w = self.last_w.get(s)
            if w is not None:
                deps.add(w)
            for r in self.readers.get(s, ()):
                deps.add(r)
        out = []
        for d in raw | deps:
            if d is op:
                continue
            if d.eng == eng and not d.dma and not dma and d not in raw and eng == "pe":
                continue
            out.append(d)
        op.deps = out
        for d in out:
            if not d.dma:
                d.sig = True
        if dma:
            n = self.ndma[eng]
            self.ndma[eng] = n + 1
            op.dsem = n % NDMASEM
            op.dval = 16 * (n // NDMASEM + 1)
            op.dprev = 16 * (n // NDMASEM)
        for s in reads:
            self.readers.setdefault(s, []).append(op)
        for s in writes:
            self.last_w[s] = op
            self.readers[s] = []
        self.q[eng].append(op)
        return op

    def emit(self, nc, stack, final_wait_ops=()):
        nsem = {}
        for e in self.ENG:
            c = 0
            for op in self.q[e]:
                if op.sig and not op.dma:
                    c += 1
                    op.sigcount = c
            nsem[e] = (c + EPOCH - 1) // EPOCH
        csem = {e: [stack.enter_context(nc.semaphore(f"c_{e}_{i}")) for i in range(nsem[e])] for e in self.ENG}
        dsem = {e: [stack.enter_context(nc.semaphore(f"d_{e}_{i}")) for i in range(NDMASEM)] if self.ndma[e] else [] for e in self.ENG}
        block = stack.enter_context(nc.Block())
        deco = {"pe": block.tensor, "act": block.scalar, "dve": block.vector, "pool": block.gpsimd, "sp": block.sync}
        stats = {}

        def make(e):
            def body(eng):
                waited_c = {p: 0 for p in self.ENG}
                waited_d = {}
                nw = 0
                for op in self.q[e]:
                    need_c = {}
                    need_d = {}
                    for d in op.deps:
                        if d.dma:
                            k = (d.eng, d.dsem)
                            if waited_d.get(k, 0) < d.dval:
                                need_d[k] = max(need_d.get(k, 0), d.dval)
                        else:
                            if waited_c[d.eng] < d.sigcount:
                                need_c[d.eng] = max(need_c.get(d.eng, 0), d.sigcount)
                    if op.dma and op.dprev > 0:
                        k = (e, op.dsem)
                        if waited_d.get(k, 0) < op.dprev:
                            need_d[k] = max(need_d.get(k, 0), op.dprev)
                    for p, cnt in need_c.items():
                        si = (cnt - 1) // EPOCH
                        eng.wait_ge(csem[p][si], (cnt - 1) % EPOCH + 1)
                        waited_c[p] = cnt
                        nw += 1
                    for k, v in need_d.items():
                        eng.wait_ge(dsem[k[0]][k[1]], v)
                        waited_d[k] = v
                        nw += 1
                    ins = getattr(eng, op.fn[0])(**op.fn[1])
                    if op.dma:
                        ins.then_inc(dsem[e][op.dsem], 16)
                    elif op.sig:
                        si = (op.sigcount - 1) // EPOCH
                        ins.then_inc(csem[e][si], 1)
                if e == "sp":
                    for d in final_wait_ops:
                        if d.dma:
                            eng.wait_ge(dsem[d.eng][d.dsem], d.dval)
                        else:
                            si = (d.sigcount - 1) // EPOCH
                            eng.wait_ge(csem[d.eng][si], (d.sigcount - 1) % EPOCH + 1)
                stats[e] = (len(self.q[e]), nw)
            return body

        for e in self.ENG:
            deco[e](make(e))
        return stats


D = 1024
LAM_INIT = 0.8 - 0.6 * math.exp(-0.3 * 0)


class Cfg:
    def __init__(self, T=4096, NH=8, stages="ABC", debug=(), NB=8, NE=16):
        self.T = T
        self.TO = T // 2
        self.NT = T // 128
        self.NTO = self.TO // 128
        self.NCH = T // 512
        self.NCHO = self.TO // 512
        self.NH = NH
        self.stages = stages
        self.NB = NB
        self.NE = NE
        self.debug = debug


def build(cfg):
    T, TO, NT, NTO, NCH, NCHO, NH = cfg.T, cfg.TO, cfg.NT, cfg.NTO, cfg.NCH, cfg.NCHO, cfg.NH
    nc = bass.Bass("TRN2", target_bir_lowering=False)
    din = lambda name, shape, dt=F32: nc.dram_tensor(name, shape, dt, kind="ExternalInput").ap()
    dout = lambda name, shape, dt=F32: nc.dram_tensor(name, shape, dt, kind="ExternalOutput").ap()
    x_d = din("x", [T, D])
    g1_d = din("g1", [1, D])
    cs_d = din("cs", [T, 64])
    gq_d = din("gq", [1, 64])
    gk_d = din("gk", [1, 64])
    lam_d = din("lamv", [4, 64])
    gsub_d = din("gsub", [1, 128])
    wqkv_d = din("wqkv", [NH, 128, 8, 384])
    cp_d = din("cp", [128, 8, 12])
    wlx_d = din("wlx", [8, 128, 8, 128])
    wlg_d = din("wlg", [8, 128, 8, 128])
    wg_d = din("wg", [8, 128, 4, 128])
    NB = cfg.NB
    NE = cfg.NE
    wao_d = din("wao", [8, 128, 8, 128])
    wlo_d = din("wlo", [8, 128, 8, 128])
    wga_d = din("wga", [8, 128, 8, 128])
    wgl_d = din("wgl", [8, 128, 8, 128])
    bg_d = din("bg", [128, 16])
    wout_d = din("wout", [128, 8, 1024])
    g2_d = din("g2", [1, D])
    wr_d = din("wr", [128, 8, 20])
    br_d = din("br", [1, 20])
    weg_d = din("weg", [16, 128, 8, 512])
    weu_d = din("weu", [16, 128, 8, 512])
    wed_d = din("wed", [16, 128, 4, 1024])
    out_d = dout("out", [TO, D])
    dbg = {}
    if "oT" in cfg.debug:
        dbg["oT"] = dout("dbg_oT", [128, NH, TO], BF16)
    if "yT" in cfg.debug:
        dbg["yT"] = dout("dbg_yT", [128, 8, TO], BF16)
    if "KT" in cfg.debug:
        dbg["KT"] = dout("dbg_KT", [128, T], BF16)
        dbg["QT"] = dout("dbg_QT", [128, TO], BF16)
        dbg["V"] = dout("dbg_V", [128, NT, 130], BF16)

    P = Prog()
    fin = []
    with ExitStack() as st:
        sb = lambda name, shape, dt: st.enter_context(nc.sbuf_tensor(name, shape, dt))
        psum = lambda name, shape, dt: st.enter_context(nc.psum_tensor(name, shape, dt))
        hT = sb("hT", [128, 8, T], BF16)
        oT = sb("oT", [128, NH, TO], BF16)

        ident = sb("ident", [128, 128], BF16)
        identf = sb("identf", [128, 128], F32)
        eps = sb("eps", [128, 1], F32)
        neghalf = sb("neghalf", [128, 32], F32)
        gq = sb("gq_s", [128, 64], F32)
        gk = sb("gk_s", [128, 64], F32)
        lamv = sb("lamv_s", [128, 4, 64], F32)
        gsub = sb("gsub_s", [128, 128], F32)
        lam_t = sb("lam_t", [128, 8], F32)
        lam_j = sb("lam_j", [128, 2, 64], F32)
        cp = sb("cp_s", [128, 8, 12], F32)
        hbias = sb("hbias", [128, 8, 4], F32)
        hc = sb("hc", [128, 8, 2], F32)
        lrt = sb("lrt", [128, 8, 2], F32)
        sT = ExitStack()
        TK = sT.enter_context(nc.sbuf_tensor("TK", [128, NT, 4, 32], F32))
        TQ = sT.enter_context(nc.sbuf_tensor("TQ", [128, NTO, 4, 32], F32))
        sA = ExitStack()
        sbA = lambda name, shape, dt: sA.enter_context(nc.sbuf_tensor(name, shape, dt))
        gain1 = sbA("gain1", [128, D], F32)
        cs = sbA("cs_s", [128, NT, 64], F32)

        P.op("pool", "memset", writes=["identf"], ap=identf[:], constant=0.0)
        P.op("pool", "affine_select", reads=["identf"], writes=["identf"], out=identf[:], in_=identf[:], pattern=[[-1, 128]],
                                                 compare_op=ALU.not_equal, fill=1.0, base=0, channel_multiplier=1)
        P.op("pool", "tensor_copy", reads=["identf"], writes=["ident"], out=ident[:], in_=identf[:])
        P.op("pool", "memset", writes=["eps"], ap=eps[:], constant=1e-6)
        P.op("pool", "memset", writes=["neghalf"], ap=neghalf[:], constant=-0.5)
        P.dma("sp", writes=["gain1"], out=gain1[:], in_=g1_d.partition_broadcast(128))
        P.dma("sp", writes=["gq"], out=gq[:], in_=gq_d.partition_broadcast(128))
        P.dma("sp", writes=["gk"], out=gk[:], in_=gk_d.partition_broadcast(128))
        P.dma("sp", writes=["gsub"], out=gsub[:], in_=gsub_d.partition_broadcast(128))
        for i in range(4):
            P.dma("sp", writes=[("lamv", i)], out=lamv[:, i, :], in_=lam_d[i:i + 1, :].partition_broadcast(128))
        P.dma("sp", writes=["cs"], out=cs[:], in_=cs_d.rearrange("(n p) f -> p n f", p=128))
        for (Tt, g, gname, n, tname) in ((TK, gk, "gk", NT, "TK"), (TQ, gq, "gq", NTO, "TQ")):
            for ti, (csoff, goff) in enumerate(((0, 0), (32, 32), (32, 0), (0, 32))):
                P.op("pool", "tensor_tensor", reads=["cs", gname], writes=[tname],
                    out=Tt[:, :, ti, :], in0=cs[:, 0:n, csoff:csoff + 32],
                    in1=g[:, goff:goff + 32].unsqueeze(1).to_broadcast([128, n, 32]), op=ALU.mult)
        for i in range(2):
            P.op("dve", "tensor_tensor", reads=[("lamv", 2 * i), ("lamv", 2 * i + 1)], writes=[("lam_j", i)], out=lam_j[:, i, :], in0=lamv[:, 2 * i, :], in1=lamv[:, 2 * i + 1, :], op=ALU.mult)
        P.op("dve", "tensor_reduce", reads=[("lam_j", 0), ("lam_j", 1)], writes=["lam_t01"], out=lam_t[:, 0:2], in_=lam_j[:], axis=AX.X, op=ALU.add)
        P.op("act", "activation", reads=["lam_t01"], writes=["lam_t23"], out=lam_t[:, 2:4], in_=lam_t[:, 0:2], func=AF.Exp)
        P.op("dve", "tensor_tensor", reads=["lam_t23"], writes=["lam_t4"], out=lam_t[:, 4:5], in0=lam_t[:, 2:3], in1=lam_t[:, 3:4], op=ALU.subtract)
        P.op("dve", "tensor_scalar", reads=["lam_t4"], writes=["neglam"], out=lam_t[:, 5:6], in0=lam_t[:, 4:5], scalar1=-1.0, scalar2=-LAM_INIT,
                                               op0=ALU.mult, op1=ALU.add)
        P.op("dve", "tensor_scalar", reads=["gsub"], writes=["gsub"], out=gsub[:], in0=gsub[:], scalar1=(1.0 - LAM_INIT), scalar2=None, op0=ALU.mult)

        xt = sbA("xt", [128, 8, D], F32)
        junk = sbA("junk", [128, D], BF16)
        ssA = sbA("ssA", [128, 2, 4], F32)
        msA = sbA("msA", [128, 2, 4], F32)
        rsA = sbA("rsA", [128, 2, 4], F32)
        hb = sbA("hb", [128, 2, 4, D], BF16)
        banks = [psum(f"bank{i}", [128, 512], F32) for i in range(8)]
        bk_bf = [b[:].bitcast(BF16) for b in banks]

        nb = 0
        for c in range(NCH):
            cb = c % 2
            for j in range(4):
                tt = c * 4 + j
                P.dma("sp", writes=[("xt", cb * 4 + j)], out=xt[:, cb * 4 + j, :], in_=x_d[tt * 128:(tt + 1) * 128, :])
                P.op("act", "activation", reads=[("xt", cb * 4 + j)], writes=["junk", ("ssA", cb, j)], out=junk[:], in_=xt[:, cb * 4 + j, :], func=AF.Square,
                                                               accum_out=ssA[:, cb, j:j + 1])
            P.op("dve", "tensor_scalar", reads=[("ssA", cb, j) for j in range(4)], writes=[("msA", cb)], out=msA[:, cb, :], in0=ssA[:, cb, :], scalar1=1.0 / D, scalar2=1e-6,
                                                         op0=ALU.mult, op1=ALU.add)
            P.op("pool", "tensor_tensor", reads=[("msA", cb), "neghalf"], writes=[("rsA", cb)], out=rsA[:, cb, :], in0=msA[:, cb, :], in1=neghalf[:, 0:4], op=ALU.pow)
            for j in range(4):
                P.op("dve", "scalar_tensor_tensor", reads=[("xt", cb * 4 + j), ("rsA", cb), "gain1"], writes=[("hb", cb, j)],
                    out=hb[:, cb, j, :], in0=xt[:, cb * 4 + j, :], scalar=rsA[:, cb, j:j + 1], in1=gain1[:],
                    op0=ALU.mult, op1=ALU.mult)
            for kp in range(4):
                bank = nb % 2
                nb += 1
                for kk in range(2):
                    k = kp * 2 + kk
                    for j in range(4):
                        P.op("pe", "transpose", reads=[("hb", cb, j), "ident"], writes=[("bank", bank)],
                            out=bk_bf[bank][:, kk * 512 + j * 128: kk * 512 + (j + 1) * 128],
                            in_=hb[:, cb, j, k * 128:(k + 1) * 128], identity=ident[:])
                P.op("act", "copy", reads=[("bank", bank)], writes=[("hT", c)],
                    out=hT[:, kp * 2:kp * 2 + 2, c * 512:(c + 1) * 512],
                    in_=bk_bf[bank].rearrange("p (a b) -> p a b", a=2))
        P.barrier()
        sA.close()

        sB = ExitStack()
        sbB = lambda name, shape, dt: sB.enter_context(nc.sbuf_tensor(name, shape, dt))
        wh = sbB("wh", [128, 2, 8, 384], BF16)
        KT = sbB("KT", [128, 2, T], BF16)
        QT = sbB("QT", [128, 2, TO], BF16)
        VA = sbB("VA", [128, 2, NT, 130], BF16)
        raw = sbB("raw", [128, 1, 4, 384], F32)
        sqb = sbB("sqb", [128, 4, 256], F32)
        ssB = sbB("ssB", [128, 4, 4], F32)
        msB = sbB("msB", [128, 4, 4], F32)
        rsB = sbB("rsB", [128, 4, 4], F32)
        tA = sbB("tA", [128, 1, 4, 2, 32], F32)
        tB = sbB("tB", [128, 1, 4, 2, 32], F32)
        rot = sbB("rot", [128, 1, 4, 2, 64], F32)
        qkb = sbB("qkb", [128, 2, 4, 128], BF16)
        PT = sbB("PT", [128, 2, 2, 512], BF16)
        rcp = sbB("rcp", [128, 3, 3], F32)
        nrcp = sbB("nrcp", [128, 3, 3], F32)
        ocp = sbB("ocp", [128, 3, 387], F32)
        osq = sqb[:, :, 0:128]
        oss = sbB("oss", [128, 4], F32)
        oms = sbB("oms", [128, 4], F32)
        ors = sbB("ors", [128, 4], F32)
        onb = sbB("onb", [128, 4, 128], BF16)

        PREP = 7
        SB = [(0, 1), (2, 3)]
        OB = [4, 5, 6]
        pS = [None, None]
        for i in range(2):
            pass
        P.op("pool", "memset", writes=[("rcp", 2)], ap=rcp[:], constant=1.0)
        for hb_i in range(2):
            P.op("pool", "memset", writes=[("VA1", hb_i)], ap=VA[:, hb_i, :, 128:130], constant=1.0)

        def slot_ap(j, s):
            idx = j * 2 + s
            b, sl = OB[idx // 3], idx % 3
            return banks[b][:, sl * 129: sl * 129 + 129], b, idx

        def prep_start(h):
            hbi = h % 2
            P.dma("pool", writes=[("wh", hbi)], out=wh[:, hbi], in_=wqkv_d[h])

        def proj_tile(h, c, j):
            hbi = h % 2
            own = c < NCHO
            lo = 0 if own else 128
            rb = 0
            if True:
                if True:
                    tt = c * 4 + j
                    pb = PREP if (h > 0 or j % 2 == 0) else SB[0][0]
                    for k in range(8):
                        P.op("pe", "matmul", reads=[("hT", c), ("wh", hbi)], writes=[("bank", pb)],
                            out=banks[pb][:, lo:384], lhsT=hT[:, k, tt * 128:(tt + 1) * 128], rhs=wh[:, hbi, k, lo:384],
                            start=(k == 0), stop=(k == 7))
                    P.op("dve", "tensor_copy", reads=[("bank", pb)], writes=[("raw", rb, j)], out=raw[:, rb, j, lo:384], in_=banks[pb][:, lo:384])

        def elem(h, c):
            hbi = h % 2
            own = c < NCHO
            lo = 0 if own else 128
            rb = 0
            if True:
                rr = [("raw", rb, j) for j in range(4)]
                ng = 4 if own else 2
                qk = raw[:, rb, :, lo:256]
                P.op("pool", "tensor_tensor", reads=rr, writes=["sqb"], out=sqb[:, :, lo:256], in0=qk, in1=qk, op=ALU.mult)
                gl = lo // 64
                P.op("dve", "tensor_reduce", reads=["sqb"], writes=["ssB"],
                    out=ssB[:, :, gl:4], in_=sqb[:, :, lo:256].rearrange("p t (g d) -> p t g d", d=64), axis=AX.X, op=ALU.add)
                P.op("dve", "tensor_scalar", reads=["ssB"], writes=["msB"], out=msB[:, :, gl:4], in0=ssB[:, :, gl:4], scalar1=1.0 / 64, scalar2=1e-6,
                                                              op0=ALU.mult, op1=ALU.add)
                P.op("pool", "tensor_tensor", reads=["msB", "neghalf"], writes=["rsB"], out=rsB[:, :, gl:4], in0=msB[:, :, gl:4],
                                                               in1=neghalf[:, 0:4 * (4 - gl)].rearrange("p (a b) -> p a b", a=4), op=ALU.pow)
                for qi in ([0, 1] if own else [1]):
                    eng = "dve"
                    xv = raw[:, rb, :, qi * 128:(qi + 1) * 128].rearrange("p t (g d) -> p t g d", d=64)
                    x1 = xv[:, :, :, 0:32]
                    x2 = xv[:, :, :, 32:64]
                    Tt = TQ if qi == 0 else TK
                    tname = "TQ" if qi == 0 else "TK"

                    def tb(ti):
                        return Tt[:, c * 4:c * 4 + 4, ti, :].unsqueeze(2).to_broadcast([128, 4, 2, 32])
                    A, B = tA[:, 0], tB[:, 0]
                    P.op(eng, "tensor_tensor", reads=rr + [tname], writes=["tA"], out=A, in0=x1, in1=tb(0), op=ALU.mult)
                    P.op(eng, "tensor_tensor", reads=rr + [tname], writes=["tB"], out=B, in0=x2, in1=tb(1), op=ALU.mult)
                    P.op(eng, "tensor_tensor", reads=["tA", "tB"], writes=["rot1"], out=rot[:, 0, :, :, 0:32], in0=A, in1=B, op=ALU.subtract)
                    P.op(eng, "tensor_tensor", reads=rr + [tname, "rot1"], writes=["tA"], out=A, in0=x1, in1=tb(2), op=ALU.mult)
                    P.op(eng, "tensor_tensor", reads=rr + [tname, "rot1"], writes=["tB"], out=B, in0=x2, in1=tb(3), op=ALU.mult)
                    P.op(eng, "tensor_tensor", reads=["tA", "tB"], writes=["rot2"], out=rot[:, 0, :, :, 32:64], in0=A, in1=B, op=ALU.add)
                    P.op(eng, "tensor_tensor", reads=["rot1", "rot2", "rsB"], writes=[("qkb", qi)],
                        out=qkb[:, qi].rearrange("p t (g d) -> p t g d", d=64), in0=rot[:, 0],
                        in1=rsB[:, :, 2 * qi:2 * qi + 2].unsqueeze(3).to_broadcast([128, 4, 2, 64]), op=ALU.mult)
                P.op("pool", "tensor_copy", reads=rr, writes=[("VA", hbi, c)], out=VA[:, hbi, c * 4:c * 4 + 4, 0:128], in_=raw[:, rb, :, 256:384])

        def trans(h, c):
            hbi = h % 2
            own = c < NCHO
            if True:
                for qi in ([0, 1] if own else [1]):
                    for j in range(4):
                        P.op("pe", "transpose", reads=[("qkb", qi), "ident"], writes=[("bank", PREP)],
                            out=bk_bf[PREP][:, (qi * 4 + j) * 128:(qi * 4 + j + 1) * 128], in_=qkb[:, qi, j, :], identity=ident[:])
                P.op("dve", "tensor_copy", reads=[("bank", PREP)], writes=[("KT", hbi, c)], out=KT[:, hbi, c * 512:(c + 1) * 512], in_=bk_bf[PREP][:, 512:1024])
                if own:
                    P.op("dve", "tensor_copy", reads=[("bank", PREP)], writes=[("QT", hbi, c)], out=QT[:, hbi, c * 512:(c + 1) * 512], in_=bk_bf[PREP][:, 0:512])

        def attn(h, inject):
            hbi = h % 2
            it = 0
            tot = NCHO * NT
            every = max(1, tot // max(1, len(inject)))
            for qc in range(NCHO):
                def qk_mm(kt, sbi):
                    kc = kt // 4
                    for s in range(2):
                        P.op("pe", "matmul", reads=[("KT", hbi, kc), ("QT", hbi, qc)], writes=[("bank", SB[sbi][s])],
                            out=banks[SB[sbi][s]][:, :], lhsT=KT[s * 64:(s + 1) * 64, hbi, kt * 128:(kt + 1) * 128],
                            rhs=QT[s * 64:(s + 1) * 64, hbi, qc * 512:(qc + 1) * 512], start=True, stop=True)

                def exp_op(kt, sbi, pbi):
                    for s in range(2):
                        P.op("act", "activation", reads=[("bank", SB[sbi][s])], writes=[("PT", pbi, s)], out=PT[:, pbi, s, :], in_=banks[SB[sbi][s]][:, :], func=AF.Exp, scale=0.125)

                def pv_mm(kt, pbi):
                    kc = kt // 4
                    for s in range(2):
                        for j in range(4):
                            oap, b, idx = slot_ap(j, s)
                            first = (kt == 0) and ((j, s) in ((0, 0), (2, 0), (3, 0)))
                            P.op("pe", "matmul", reads=[("PT", pbi, s), ("VA", hbi, kc), ("VA1", hbi)], writes=[("bank", b)],
                                out=oap, lhsT=PT[:, pbi, s, j * 128:(j + 1) * 128], rhs=VA[:, hbi, kt, 0:129],
                                start=first, stop=(kt == NT - 1), skip_group_check=True)

                for kt in range(NT + 1):
                    if kt < NT:
                        qk_mm(kt, (it + kt) % 2)
                        exp_op(kt, (it + kt) % 2, (it + kt) % 2)
                    if kt >= 1:
                        pv_mm(kt - 1, (it + kt - 1) % 2)
                    if kt == min(8, NT - 1) and deferred:
                        deferred.pop(0)()
                    if kt < NT and (it + kt) % every == 0 and inject:
                        inject.pop(0)()
                it += NT
                ob = [("bank", b) for b in OB]
                for bi in range(3):
                    nsl = 3 if bi < 2 else 2
                    P.op("dve", "tensor_copy", reads=[ob[bi]], writes=[("ocp", bi)], out=ocp[:, bi, 0:129 * nsl], in_=banks[OB[bi]][:, 0:129 * nsl])
                for bi in range(3):
                    nsl = 3 if bi < 2 else 2
                    P.op("dve", "reciprocal", reads=[("ocp", bi)], writes=[("rcp", bi)], out=rcp[:, bi, 0:nsl],
                         in_=ocp[:, bi, 128:128 + 129 * (nsl - 1) + 1:129])
                P.op("dve", "tensor_scalar", reads=[("rcp", 0), ("rcp", 1), ("rcp", 2), "neglam"], writes=["nrcp"], out=nrcp[:], in0=rcp[:], scalar1=lam_t[:, 5:6], scalar2=None, op0=ALU.mult)

                def oslot(j, s_):
                    idx = j * 2 + s_
                    return ocp[:, idx // 3, (idx % 3) * 129:(idx % 3) * 129 + 128], idx
                for j in range(4):
                    o1, i1 = oslot(j, 0)
                    o2, i2 = oslot(j, 1)
                    P.op("dve", "tensor_scalar", reads=[("ocp", i1 // 3), ("rcp", i1 // 3)], writes=[("ocp", i1 // 3)],
                        out=o1, in0=o1, scalar1=rcp[:, i1 // 3, i1 % 3:i1 % 3 + 1], scalar2=None, op0=ALU.mult)
                    P.op("dve", "scalar_tensor_tensor", reads=[("ocp", i2 // 3), ("ocp", i1 // 3), "nrcp"], writes=[("ocp", i1 // 3)],
                        out=o1, in0=o2, scalar=nrcp[:, i2 // 3, i2 % 3:i2 % 3 + 1], in1=o1, op0=ALU.mult, op1=ALU.add)
                oc = [("ocp", bi) for bi in range(3)]
                for j in range(4):
                    o1, i1 = oslot(j, 0)
                    P.op("pool", "tensor_tensor", reads=oc, writes=["sqb"], out=osq[:, j, :], in0=o1, in1=o1, op=ALU.mult)
                P.op("dve", "tensor_reduce", reads=["sqb"], writes=["oss"], out=oss[:], in_=osq, axis=AX.X, op=ALU.add)
                P.op("dve", "tensor_scalar", reads=["oss"], writes=["oms"], out=oms[:], in0=oss[:], scalar1=1.0 / 128, scalar2=1e-6, op0=ALU.mult, op1=ALU.add)
                P.op("pool", "tensor_tensor", reads=["oms", "neghalf"], writes=["ors"], out=ors[:], in0=oms[:], in1=neghalf[:, 0:4], op=ALU.pow)
                for j in range(4):
                    o1, i1 = oslot(j, 0)
                    P.op("dve", "scalar_tensor_tensor", reads=oc + ["ors", "gsub"], writes=[("onb", j)],
                        out=onb[:, j, :], in0=o1, scalar=ors[:, j:j + 1], in1=gsub[:], op0=ALU.mult, op1=ALU.mult)

                def fin_o(h=h, qc=qc):
                    for j in range(4):
                        P.op("pe", "transpose", reads=[("onb", j), "ident"], writes=[("bank", PREP)], out=bk_bf[PREP][:, j * 128:(j + 1) * 128], in_=onb[:, j, :], identity=ident[:])
                    P.op("act", "copy", reads=[("bank", PREP)], writes=[("oT", h, qc)], out=oT[:, h, qc * 512:(qc + 1) * 512], in_=bk_bf[PREP][:, 0:512])
                deferred.append(fin_o)

        def prep_steps(h):
            steps = []
            for c in range(NCH):
                for j in range(4):
                    if j < 3:
                        steps.append(lambda c=c, j=j: proj_tile(h, c, j))
                    elif c > 0:
                        steps.append(lambda c=c, j=j: (trans(h, c - 1), proj_tile(h, c, j), elem(h, c)))
                    else:
                        steps.append(lambda c=c, j=j: (proj_tile(h, c, j), elem(h, c)))
            steps.append(lambda: trans(h, NCH - 1))
            return steps

        deferred = []
        prep_start(0)
        for f in prep_steps(0):
            f()
        for h in range(NH):
            inj = []
            if h + 1 < NH:
                prep_start(h + 1)
                inj = prep_steps(h + 1)
            attn(h, inj)
            while inj:
                inj.pop(0)()
            if h == NH - 1:
                while deferred:
                    deferred.pop(0)()
            if "KT" in dbg and h == NH - 1:
                hbi = h % 2
                fin.append(P.dma("sp", reads=[("KT", hbi, c) for c in range(NCH)], out=dbg["KT"], in_=KT[:, hbi, :]))
                fin.append(P.dma("sp", reads=[("QT", hbi, c) for c in range(NCHO)], out=dbg["QT"], in_=QT[:, hbi, :]))
                fin.append(P.dma("sp", reads=[("VA", hbi, c) for c in range(NCH)] + [("VA1", hbi)], out=dbg["V"], in_=VA[:, hbi]))
        if "oT" in dbg:
            fin.append(P.dma("sp", reads=[("oT", h, qc) for h in range(NH) for qc in range(NCHO)], out=dbg["oT"], in_=oT[:]))
        P.barrier()
        sB.close()
        sT.close()

        P.dma("sp", writes=["cp"], out=cp[:], in_=cp_d)
        P.op("dve", "tensor_scalar", reads=["cp"], writes=["hbias"], out=hbias[:], in0=cp[:, :, 6:10], scalar1=0.5, scalar2=None, op0=ALU.mult)
        P.op("act", "activation", reads=["cp"], writes=["lrt"], out=lrt[:], in_=cp[:, :, 10:12], func=AF.Exp, scale=-1.0)
        P.op("act", "activation", reads=["lrt"], writes=["lrt2"], out=lrt[:], in_=lrt[:], func=AF.Ln, bias=1.0)
        P.op("dve", "tensor_scalar", reads=["lrt2"], writes=["hc"], out=hc[:], in0=lrt[:], scalar1=-4.0, scalar2=None, op0=ALU.mult)
        yT = sb("yT", [128, 8, TO], BF16)
        sL = ExitStack()
        sbL = lambda name, shape, dt: sL.enter_context(nc.sbuf_tensor(name, shape, dt))
        wl = sbL("wl_s", [128, 2, 2, 8, 128], BF16)
        wgt = sbL("wgt", [128, 2, 4, 128], BF16)
        xs = sbL("xs", [128, TO + 4], F32)
        xr = sbL("xr", [128, TO], F32)
        xrb = sbL("xrb", [128, TO], BF16)
        Ab = sbL("Ab", [128, TO], F32)
        Ub = sbL("Ub", [128, TO], F32)
        T1 = sbL("T1", [128, TO], F32)
        hA = sbL("hA", [128, TO], F32)
        gg = sbL("gg", [128, TO], F32)
        stB = sbL("stB", [128, 1], F32)
        NCL = TO // 512
        cnt = {"p": 0, "g": 0}
        g2 = T1

        def ck(name):
            return [(name, ch) for ch in range(NCL)]

        def proj_pieces(widx, buf, tok_lo, ntok, dst, dst_col, dkeys, evac_eng):
            pieces = []
            done = 0
            while done < ntok:
                w = min(512, ntok - done)

                def piece(done=done, w=w):
                    bank = cnt["p"] % 4
                    cnt["p"] += 1
                    lo = tok_lo + done
                    for k in range(8):
                        P.op("pe", "matmul", reads=[("hT", lo // 512), ("hT", (lo + w - 1) // 512), ("wl", buf, widx)], writes=[("bank", bank)],
                             out=banks[bank][:, 0:w], lhsT=wl[:, buf, widx, k, :], rhs=hT[:, k, lo:lo + w], start=(k == 0), stop=(k == 7))
                    if evac_eng == "act":
                        P.op("act", "copy", reads=[("bank", bank)], writes=dkeys(dst_col + done, dst_col + done + w), out=dst[:, dst_col + done:dst_col + done + w], in_=banks[bank][:, 0:w])
                    else:
                        P.op("dve", "tensor_copy", reads=[("bank", bank)], writes=dkeys(dst_col + done, dst_col + done + w), out=dst[:, dst_col + done:dst_col + done + w], in_=banks[bank][:, 0:w])
                pieces.append(piece)
                done += w
            return pieces

        def xskeys(c0, c1):
            return [("xs", i) for i in range(c0 // 512, (c1 - 1) // 512 + 1)]

        def ggkeys(c0, c1):
            return [("gg", i) for i in range(c0 // 512, (c1 - 1) // 512 + 1)]

        def conv_chunk(n, ch):
            lo = ch * 512
            xk = xskeys(lo, lo + 516)
            P.op("dve", "tensor_scalar", reads=xk + ["cp"], writes=[("xr", ch)], out=xr[:, lo:lo + 512], in0=xs[:, lo:lo + 512], scalar1=cp[:, n, 0:1], scalar2=cp[:, n, 5:6],
                 op0=ALU.mult, op1=ALU.add)
            for j in range(1, 5):
                P.op("dve", "scalar_tensor_tensor", reads=xk + ["cp", ("xr", ch)], writes=[("xr", ch)], out=xr[:, lo:lo + 512], in0=xs[:, lo + j:lo + j + 512], scalar=cp[:, n, j:j + 1],
                     in1=xr[:, lo:lo + 512], op0=ALU.mult, op1=ALU.add)
            P.op("pool", "tensor_copy", reads=[("xr", ch)], writes=[("xrb", ch)], out=xrb[:, lo:lo + 512], in_=xr[:, lo:lo + 512])

        def front_end(n, phase):
            buf = n % 2
            if phase == "O":
                first = [lambda: P.op("pool", "memset", reads=[], writes=xskeys(TO + 2, TO + 4), ap=xs[:, TO + 2:TO + 4], constant=0.0)]
                pcs = proj_pieces(0, buf, TO - 2, TO + 2, xs, 0, xskeys, "dve")
            else:
                first = [lambda: P.op("pool", "memset", reads=[], writes=xskeys(0, 2), ap=xs[:, 0:2], constant=0.0)]
                pcs = proj_pieces(0, buf, 0, TO + 2, xs, 2, xskeys, "dve")
            pcs = first + pcs
            state = {"i": 0}

            def hook(ch):
                need = len(pcs) if ch == NCL - 1 else min(len(pcs), ch + 3)
                while state["i"] < need:
                    pcs[state["i"]]()
                    state["i"] += 1
                conv_chunk(n, ch)
            return hook

        def lru_dir(n, buf, d, reverse, init, init_reads, out, okey, hook=None):
            def gates(ch):
                sl = slice(ch * 512, (ch + 1) * 512)
                bx = 4 + cnt["g"] % 4
                by = 4 + (cnt["g"] + 1) % 4
                cnt["g"] += 2
                P.op("pe", "matmul", reads=[("xrb", ch), ("wgt", buf)], writes=[("bank", bx)], out=banks[bx][:, :], lhsT=wgt[:, buf, 2 * d, :], rhs=xrb[:, sl], start=True, stop=True)
                P.op("pe", "matmul", reads=[("xrb", ch), ("wgt", buf)], writes=[("bank", by)], out=banks[by][:, :], lhsT=wgt[:, buf, 2 * d + 1, :], rhs=xrb[:, sl], start=True, stop=True)
                return bx, by
            nxt = gates(0)
            for ch in range(NCL):
                sl = slice(ch * 512, (ch + 1) * 512)
                bx, by = nxt
                P.op("act", "activation", reads=[("bank", bx), "hbias"], writes=[("T1", ch)], out=T1[:, sl], in_=banks[bx][:, :], func=AF.Tanh, scale=0.5, bias=hbias[:, n, d:d + 1])
                P.op("act", "activation", reads=[("bank", by), "hbias"], writes=[("U", ch)], out=Ub[:, sl], in_=banks[by][:, :], func=AF.Tanh, scale=0.5, bias=hbias[:, n, 2 + d:3 + d])
                P.op("act", "activation", reads=[("T1", ch), "hc"], writes=[("A", ch)], out=Ab[:, sl], in_=T1[:, sl], func=AF.Exp, scale=hc[:, n, d:d + 1], bias=hc[:, n, d:d + 1])
                if ch + 1 < NCL:
                    nxt = gates(ch + 1)
                P.op("pool", "tensor_scalar", reads=[("A", ch)], writes=[("A", ch)], out=Ab[:, sl], in0=Ab[:, sl], scalar1=1.0, scalar2=-3.0e38, op0=ALU.min, op1=ALU.max)
                P.op("pool", "tensor_tensor", reads=[("A", ch)], writes=[("T1", ch)], out=T1[:, sl], in0=Ab[:, sl], in1=Ab[:, sl], op=ALU.mult)
                P.op("dve", "scalar_tensor_tensor", reads=[("U", ch), ("xr", ch)], writes=[("U", ch)], out=Ub[:, sl], in0=Ub[:, sl], scalar=1.0, in1=xr[:, sl], op0=ALU.add, op1=ALU.mult)
                if hook is not None:
                    hook(ch)
            P.op("act", "activation", reads=ck("T1"), writes=ck("T1"), out=T1[:], in_=T1[:], func=AF.Sqrt, scale=-1.0, bias=1.0)
            P.op("dve", "scalar_tensor_tensor", reads=ck("U") + ck("T1"), writes=ck("U"), out=Ub[:], in0=Ub[:], scalar=0.5, in1=T1[:], op0=ALU.mult, op1=ALU.mult)
            if reverse:
                P.op("dve", "tensor_tensor_scan", reads=ck("A") + ck("U") + init_reads, writes=okey, out=out[:, ::-1], data0=Ab[:, ::-1], data1=Ub[:, ::-1], initial=init,
                     op0=ALU.mult, op1=ALU.add)
            else:
                P.op("dve", "tensor_tensor_scan", reads=ck("A") + ck("U") + init_reads, writes=okey, out=out[:], data0=Ab[:], data1=Ub[:], initial=init, op0=ALU.mult, op1=ALU.add)

        def l_weights(n):
            buf = n % 2
            P.dma("pool", writes=[("wl", buf, 0)], out=wl[:, buf, 0], in_=wlx_d[n])
            P.dma("pool", writes=[("wl", buf, 1)], out=wl[:, buf, 1], in_=wlg_d[n])
            P.dma("pool", writes=[("wgt", buf)], out=wgt[:, buf], in_=wg_d[n])

        def l_mid(n):
            buf = n % 2
            if n + 1 < NB:
                l_weights(n + 1)
            lru_dir(n, buf, 1, True, 0.0, [], Ab, ck("A"), hook=front_end(n, "W"))
            P.op("dve", "tensor_copy", reads=ck("A"), writes=["stB"], out=stB[:], in_=Ab[:, 0:1])
            gpcs = proj_pieces(1, buf, 0, TO, gg, 0, ggkeys, "dve")
            lru_dir(n, buf, 0, False, 0.0, [], hA, ["hA"], hook=lambda ch: gpcs[ch]() if ch < len(gpcs) else None)
            for f in gpcs[NCL:]:
                f()
            nxt = front_end(n + 1, "O") if n + 1 < NB else None
            lru_dir(n, buf, 1, True, stB[:, 0:1], ["stB"], Ab, ck("A"), hook=nxt)

        def l_tail(n):
            P.op("pool", "tensor_tensor", reads=["hA"] + ck("A"), writes=["hA"], out=hA[:], in0=hA[:], in1=Ab[:], op=ALU.add)
            P.op("pool", "tensor_tensor", reads=ck("gg"), writes=ck("T1"), out=g2[:], in0=gg[:], in1=gg[:], op=ALU.mult)
            P.op("pool", "tensor_scalar", reads=ck("T1"), writes=ck("T1"), out=g2[:], in0=g2[:], scalar1=0.044715, scalar2=1.0, op0=ALU.mult, op1=ALU.add)
            P.op("dve", "tensor_tensor", reads=ck("T1") + ck("gg"), writes=ck("T1"), out=g2[:], in0=g2[:], in1=gg[:], op=ALU.mult)
            P.op("act", "activation", reads=ck("T1"), writes=ck("T1"), out=g2[:], in_=g2[:], func=AF.Tanh, scale=0.7978845608028654)
            P.op("dve", "scalar_tensor_tensor", reads=ck("T1") + ck("gg"), writes=ck("T1"), out=g2[:], in0=g2[:], scalar=1.0, in1=gg[:], op0=ALU.add, op1=ALU.mult)
            P.op("dve", "scalar_tensor_tensor", reads=["hA"] + ck("T1"), writes=[("yT", n)], out=yT[:, n, :], in0=hA[:], scalar=0.5, in1=g2[:], op0=ALU.mult, op1=ALU.mult)

        l_weights(0)
        h0 = front_end(0, "O")
        for ch in range(NCL):
            h0(ch)
        for n in range(NB):
            l_mid(n)
            l_tail(n)
        if "yT" in dbg:
            fin.append(P.dma("sp", reads=[("yT", n) for n in range(NB)], out=dbg["yT"], in_=yT[:]))
        P.barrier()
        sL.close()

        sM = ExitStack()
        sbM = lambda name, shape, dt: sM.enter_context(nc.sbuf_tensor(name, shape, dt))
        mT = sbM("mT", [128, 8, TO], BF16)
        cw = sbM("cw", [128, NTO, 16], F32)
        bg = sbM("bg_s", [128, 16], F32)
        gain2 = sbM("gain2", [128, D], F32)
        wr32 = sbM("wr32", [128, 8, 20], F32)
        wrb = sbM("wrb", [128, 8, 20], BF16)
        brb = sbM("brb", [128, 20], F32)
        sM1 = ExitStack()
        sbM1 = lambda name, shape, dt: sM1.enter_context(nc.sbuf_tensor(name, shape, dt))
        wm = sbM1("wm_s", [128, 2, 4, 8, 128], BF16)
        gsb = sbM1("gsb", [128, 2, 2, 512], F32)
        P.dma("sp", writes=["bg"], out=bg[:], in_=bg_d)
        P.dma("sp", writes=["gain2"], out=gain2[:], in_=g2_d.partition_broadcast(128))
        P.dma("sp", writes=["wr32"], out=wr32[:], in_=wr_d)
        P.dma("sp", writes=["brb"], out=brb[:], in_=br_d.partition_broadcast(128))
        P.op("pool", "tensor_copy", reads=["wr32"], writes=["wrb"], out=wrb[:], in_=wr32[:])
        it = 0
        for ec in range(8):
            buf = ec % 2
            for wi_, wd_ in enumerate((wao_d, wlo_d, wga_d, wgl_d)):
                P.dma("pool", writes=[("wm", buf, wi_)], out=wm[:, buf, wi_], in_=wd_[ec])
            for tc in range(NCHO):
                st_ = it % 2
                it += 1
                bb = [4 * st_ + i for i in range(4)]
                tsl = slice(tc * 512, (tc + 1) * 512)
                srcs = [(oT, [("oT", h, tc) for h in range(NH)]), (yT, [("yT", n) for n in range(NB)]), (hT, [("hT", tc)]), (hT, [("hT", tc)])]
                for wi_ in range(4):
                    src, rk = srcs[wi_]
                    for k in range(8):
                        P.op("pe", "matmul", reads=rk + [("wm", buf, wi_)], writes=[("bank", bb[wi_])], out=banks[bb[wi_]][:, :],
                             lhsT=wm[:, buf, wi_, k, :], rhs=src[:, k, tsl], start=(k == 0), stop=(k == 7))
                P.op("act", "activation", reads=[("bank", bb[2]), "bg"], writes=[("gsb", st_, 0)], out=gsb[:, st_, 0, :], in_=banks[bb[2]][:, :], func=AF.Sigmoid, bias=bg[:, ec:ec + 1])
                P.op("act", "activation", reads=[("bank", bb[3]), "bg"], writes=[("gsb", st_, 1)], out=gsb[:, st_, 1, :], in_=banks[bb[3]][:, :], func=AF.Sigmoid, bias=bg[:, 8 + ec:9 + ec])
                P.op("dve", "tensor_tensor", reads=[("gsb", st_, 0), ("bank", bb[0])], writes=[("gsb", st_, 0)], out=gsb[:, st_, 0, :], in0=gsb[:, st_, 0, :], in1=banks[bb[0]][:, :], op=ALU.mult)
                P.op("dve", "tensor_tensor", reads=[("gsb", st_, 1), ("bank", bb[1])], writes=[("gsb", st_, 1)], out=gsb[:, st_, 1, :], in0=gsb[:, st_, 1, :], in1=banks[bb[1]][:, :], op=ALU.mult)
                P.op("dve", "tensor_tensor", reads=[("gsb", st_, 0), ("gsb", st_, 1)], writes=[("mT", ec, tc)], out=mT[:, ec, tsl], in0=gsb[:, st_, 0, :], in1=gsb[:, st_, 1, :], op=ALU.add)
        P.barrier()
        sM1.close()
        x1 = hT[:].rearrange("p a b -> p (a b)").bitcast(F32).rearrange("p (a b) -> p a b", b=D)
        h2T = oT
        sM2 = ExitStack()
        sbM2 = lambda name, shape, dt: sM2.enter_context(nc.sbuf_tensor(name, shape, dt))
        wo = sbM2("wo_s", [128, 8, D], BF16)
        xt2 = sbM2("xt2", [128, 2, D], F32)
        junk2 = sbM2("junk2", [128, D], BF16)
        ss2 = sbM2("ss2", [128, 4], F32)
        ms2 = sbM2("ms2", [128, 4], F32)
        rs2 = sbM2("rs2", [128, 4], F32)
        h2b = sbM2("h2b", [128, 4, D], BF16)
        lg = sbM2("lg", [128, 20], F32)
        rt = sbM2("rt", [128, 352], F32)
        P.dma("pool", writes=["wo"], out=wo[:], in_=wout_d)
        for c in range(NCHO):
            for j in range(4):
                tt = c * 4 + j
                xb = tt % 2
                P.dma("sp", writes=[("xt2", xb)], out=xt2[:, xb, :], in_=x_d[tt * 128:(tt + 1) * 128, :])
                for half in range(2):
                    bank = (tt * 2 + half) % 4
                    for ec in range(8):
                        P.op("pe", "matmul", reads=[("mT", ec, c), "wo"], writes=[("bank", bank)], out=banks[bank][:, :],
                             lhsT=mT[:, ec, tt * 128:(tt + 1) * 128], rhs=wo[:, ec, half * 512:(half + 1) * 512], start=(ec == 0), stop=(ec == 7))
                    P.op("dve", "tensor_tensor", reads=[("bank", bank), ("xt2", xb)], writes=[("x1", tt)], out=x1[:, tt, half * 512:(half + 1) * 512],
                         in0=xt2[:, xb, half * 512:(half + 1) * 512], in1=banks[bank][:, :], op=ALU.add)
                P.op("act", "activation", reads=[("x1", tt)], writes=["junk2", ("ss2", j)], out=junk2[:], in_=x1[:, tt, :], func=AF.Square, accum_out=ss2[:, j:j + 1])
            P.op("dve", "tensor_scalar", reads=[("ss2", j) for j in range(4)], writes=["ms2"], out=ms2[:], in0=ss2[:], scalar1=1.0 / D, scalar2=1e-6, op0=ALU.mult, op1=ALU.add)
            P.op("pool", "tensor_tensor", reads=["ms2", "neghalf"], writes=["rs2"], out=rs2[:], in0=ms2[:], in1=neghalf[:, 0:4], op=ALU.pow)
            for j in range(4):
                tt = c * 4 + j
                P.op("dve", "scalar_tensor_tensor", reads=[("x1", tt), "rs2", "gain2"], writes=[("h2b", j)], out=h2b[:, j, :], in0=x1[:, tt, :], scalar=rs2[:, j:j + 1], in1=gain2[:],
                     op0=ALU.mult, op1=ALU.mult)
            for kp in range(4):
                bank = 4 + kp % 2
                for kk in range(2):
                    k = kp * 2 + kk
                    for j in range(4):
                        P.op("pe", "transpose", reads=[("h2b", j), "ident"], writes=[("bank", bank)],
                             out=bk_bf[bank][:, kk * 512 + j * 128: kk * 512 + (j + 1) * 128], in_=h2b[:, j, k * 128:(k + 1) * 128], identity=ident[:])
                P.op("act", "copy", reads=[("bank", bank)], writes=[("h2T", c)], out=h2T[:, kp * 2:kp * 2 + 2, c * 512:(c + 1) * 512],
                     in_=bk_bf[bank].rearrange("p (a b) -> p a b", a=2))
            for j in range(4):
                tt = c * 4 + j
                for k in range(8):
                    P.op("pe", "matmul", reads=[("h2T", c), "wrb"], writes=[("bank", 6)], out=banks[6][:, j * 20:(j + 1) * 20], lhsT=h2T[:, k, tt * 128:(tt + 1) * 128], rhs=wrb[:, k, :],
                         start=(k == 0), stop=(k == 7), skip_group_check=True)
            lg4 = rt[:, 0:80].rearrange("p (t f) -> p t f", f=20)
            P.op("dve", "tensor_tensor", reads=[("bank", 6), "brb"], writes=["lg4"], out=lg4, in0=banks[6][:, 0:80].rearrange("p (t f) -> p t f", f=20),
                 in1=brb[:].unsqueeze(1).to_broadcast([128, 4, 20]), op=ALU.add)
            glog = lg4[:, :, 0:4]
            elog = lg4[:, :, 4:20].rearrange("p t (g e) -> p t g e", e=4)
            V = lambda o, n: rt[:, o:o + n]
            V3 = lambda o: rt[:, o:o + 16].rearrange("p (t e) -> p t e", e=4)
            bc3 = lambda ap4: ap4.unsqueeze(2).to_broadcast([128, 4, 4])
            gmax, gsh, gexp, gsum, gw, goh = V(80, 4), V3(84), V3(100), V(116, 4), V(120, 4), V3(124)
            tmp4 = rt[:, 140:204].rearrange("p (t g e) -> p t g e", g=4, e=4)
            sel, m1, oh1, sel2, m2, oh2 = V3(204), V(220, 4), V3(224), V3(240), V(256, 4), V3(260)
            dd, e2, den, w1, w2, t1, t2, cwe = V(276, 4), V(280, 4), V(284, 4), V(288, 4), V(292, 4), V3(296), V3(312), V3(328)
            P.op("dve", "tensor_reduce", reads=["lg4"], writes=["gmax"], out=gmax, in_=glog, axis=AX.X, op=ALU.max)
            P.op("dve", "tensor_tensor", reads=["lg4", "gmax"], writes=["goh"], out=goh, in0=glog, in1=bc3(gmax), op=ALU.is_equal)
            P.op("dve", "tensor_tensor", reads=["lg4", "gmax"], writes=["gsh"], out=gsh, in0=glog, in1=bc3(gmax), op=ALU.subtract)
            P.op("act", "activation", reads=["gsh"], writes=["gexp"], out=gexp, in_=gsh, func=AF.Exp)
            P.op("dve", "tensor_reduce", reads=["gexp"], writes=["gsum"], out=gsum, in_=gexp, axis=AX.X, op=ALU.add)
            P.op("dve", "reciprocal", reads=["gsum"], writes=["gw"], out=gw, in_=gsum)
            P.op("dve", "tensor_tensor", reads=["lg4", "goh"], writes=["tmp4"], out=tmp4, in0=elog, in1=goh.unsqueeze(3).to_broadcast([128, 4, 4, 4]), op=ALU.mult)
            P.op("dve", "tensor_reduce", reads=["tmp4"], writes=["sel"], out=sel, in_=tmp4.rearrange("p t g e -> p t e g"), axis=AX.X, op=ALU.add)
            P.op("dve", "tensor_reduce", reads=["sel"], writes=["m1"], out=m1, in_=sel, axis=AX.X, op=ALU.max)
            P.op("dve", "tensor_tensor", reads=["sel", "m1"], writes=["oh1"], out=oh1, in0=sel, in1=bc3(m1), op=ALU.is_equal)
            P.op("dve", "scalar_tensor_tensor", reads=["oh1", "sel"], writes=["sel2"], out=sel2, in0=oh1, scalar=-1e30, in1=sel, op0=ALU.mult, op1=ALU.add)
            P.op("dve", "tensor_reduce", reads=["sel2"], writes=["m2"], out=m2, in_=sel2, axis=AX.X, op=ALU.max)
            P.op("dve", "tensor_tensor", reads=["sel2", "m2"], writes=["oh2"], out=oh2, in0=sel2, in1=bc3(m2), op=ALU.is_equal)
            P.op("dve", "tensor_tensor", reads=["m2", "m1"], writes=["dd"], out=dd, in0=m2, in1=m1, op=ALU.subtract)
            P.op("act", "activation", reads=["dd"], writes=["e2"], out=e2, in_=dd, func=AF.Exp)
            P.op("dve", "tensor_scalar", reads=["e2"], writes=["den"], out=den, in0=e2, scalar1=1.0, scalar2=None, op0=ALU.add)
            P.op("dve", "reciprocal", reads=["den"], writes=["w1a"], out=w1, in_=den)
            P.op("dve", "tensor_tensor", reads=["w1a", "gw"], writes=["w1"], out=w1, in0=w1, in1=gw, op=ALU.mult)
            P.op("dve", "tensor_tensor", reads=["w1", "e2"], writes=["w2"], out=w2, in0=w1, in1=e2, op=ALU.mult)
            P.op("dve", "tensor_tensor", reads=["oh1", "w1"], writes=["t1"], out=t1, in0=oh1, in1=bc3(w1), op=ALU.mult)
            P.op("dve", "tensor_tensor", reads=["oh2", "w2"], writes=["t2"], out=t2, in0=oh2, in1=bc3(w2), op=ALU.mult)
            P.op("dve", "tensor_tensor", reads=["t1", "t2"], writes=["cwe"], out=cwe, in0=t1, in1=t2, op=ALU.add)
            P.op("dve", "tensor_tensor", reads=["goh", "cwe"], writes=[("cw", c * 4 + j) for j in range(4)],
                 out=cw[:, c * 4:c * 4 + 4, :].rearrange("p t (g e) -> p t g e", e=4),
                 in0=goh.unsqueeze(3).to_broadcast([128, 4, 4, 4]), in1=cwe.unsqueeze(2).to_broadcast([128, 4, 4, 4]), op=ALU.mult)
        P.barrier()
        sM2.close()

        sE = ExitStack()
        sbE = lambda name, shape, dt: sE.enter_context(nc.sbuf_tensor(name, shape, dt))
        if TO >= 2048:
            yflat = yT[:].rearrange("p a b -> p (a b)")
            mflat = mT[:].rearrange("p a b -> p (a b)")
            weg = yflat[:, 0:8192].rearrange("p (u k f) -> p u k f", u=2, k=8)
            weu = yflat[:, 8192:16384].rearrange("p (u k f) -> p u k f", u=2, k=8)
            wed = mflat[:, 0:8192].rearrange("p (u f d) -> p u f d", u=2, f=4)
            hid = mflat[:, 8192:12288].rearrange("p (u f t) -> p u f t", u=2, f=4)
        else:
            weg = sbE("weg_s", [128, 2, 8, 512], BF16)
            weu = sbE("weu_s", [128, 2, 8, 512], BF16)
            wed = sbE("wed_s", [128, 2, 4, D], BF16)
            hid = sbE("hid", [128, 2, 4, 512], BF16)
        sg = sbE("sg", [128, 2, 512], F32)
        itE = 0
        for e_ in range(NE):
            buf = e_ % 2
            P.dma("pool", writes=[("weg", buf)], out=weg[:, buf], in_=weg_d[e_])
            P.dma("pool", writes=[("weu", buf)], out=weu[:, buf], in_=weu_d[e_])
            P.dma("pool", writes=[("wed", buf)], out=wed[:, buf], in_=wed_d[e_])
            for tc in range(NCHO):
                hb_ = itE % 2
                itE += 1
                tsl = slice(tc * 512, (tc + 1) * 512)
                for fc in range(4):
                    gb = (fc % 2) * 2
                    ub = gb + 1
                    for k in range(8):
                        P.op("pe", "matmul", reads=[("h2T", tc), ("weg", buf)], writes=[("bank", gb)], out=banks[gb][:, :],
                             lhsT=weg[:, buf, k, fc * 128:(fc + 1) * 128], rhs=h2T[:, k, tsl], start=(k == 0), stop=(k == 7))
                    for k in range(8):
                        P.op("pe", "matmul", reads=[("h2T", tc), ("weu", buf)], writes=[("bank", ub)], out=banks[ub][:, :],
                             lhsT=weu[:, buf, k, fc * 128:(fc + 1) * 128], rhs=h2T[:, k, tsl], start=(k == 0), stop=(k == 7))
                    P.op("act", "activation", reads=[("bank", gb)], writes=[("sg", fc % 2)], out=sg[:, fc % 2, :], in_=banks[gb][:, :], func=AF.Silu)
                    P.op("dve", "tensor_tensor", reads=[("sg", fc % 2), ("bank", ub)], writes=[("hid", hb_, fc)], out=hid[:, hb_, fc, :], in0=sg[:, fc % 2, :], in1=banks[ub][:, :], op=ALU.mult)
                for tj in range(4):
                    tt = tc * 4 + tj
                    for half in range(2):
                        db = 4 + (tj * 2 + half) % 4
                        for fc in range(4):
                            P.op("pe", "matmul", reads=[("hid", hb_, fc), ("wed", buf)], writes=[("bank", db)], out=banks[db][:, :],
                                 lhsT=hid[:, hb_, fc, tj * 128:(tj + 1) * 128], rhs=wed[:, buf, fc, half * 512:(half + 1) * 512], start=(fc == 0), stop=(fc == 3))
                        P.op("dve", "scalar_tensor_tensor", reads=[("bank", db), ("cw", tt), ("x1", tt)], writes=[("x1", tt)], out=x1[:, tt, half * 512:(half + 1) * 512],
                             in0=banks[db][:, :], scalar=cw[:, tt, e_:e_ + 1], in1=x1[:, tt, half * 512:(half + 1) * 512], op0=ALU.mult, op1=ALU.add)
                    if e_ == NE - 1:
                        fin.append(P.dma("sp", reads=[("x1", tt)], out=out_d[tt * 128:(tt + 1) * 128, :], in_=x1[:, tt, :]))
        sE.close()
        sM.close()

        stats = P.emit(nc, st, final_wait_ops=fin)
        print("stats", stats)
    return nc


_NC_CACHE = {}


def _blk(W):
    return np.ascontiguousarray(W.reshape(8, 128, 8, 128).transpose(2, 1, 0, 3))


def _pc(v):
    return v.reshape(8, 128).T


def make_in_maps(inp, T):
    f32 = np.float32
    B = inp["x"].shape[0]
    w_in = np.asarray(inp["w_in"][0], f32)
    inv = 10000.0 ** (-np.arange(0, 64, 2, dtype=np.float64) / 64.0)
    pos = np.arange(T, dtype=np.float64)
    ang = pos[:, None] * inv[None, :]
    cs_f = np.concatenate([np.cos(ang), np.sin(ang)], 1).astype(f32)
    wqkv = np.zeros((8, 128, 8, 384), f32)
    for i in range(3):
        Wp = w_in[:, i * 1024:(i + 1) * 1024].reshape(8, 128, 8, 128)
        wqkv[:, :, :, i * 128:(i + 1) * 128] = Wp.transpose(2, 1, 0, 3)
    shared = {
        "g1": np.asarray(inp["norm1_gain"][0][None, :], f32),
        "gq": np.asarray(inp["q_norm_gain"][0][None, :], f32),
        "gk": np.asarray(inp["k_norm_gain"][0][None, :], f32),
        "lamv": np.stack([inp["lambda_q1"][0], inp["lambda_k1"][0], inp["lambda_q2"][0], inp["lambda_k2"][0]]).astype(f32),
        "gsub": np.asarray(inp["attn_subln_gain"][0][None, :], f32),
        "wqkv": wqkv,
        "wlx": _blk(w_in[:, 3072:4096]), "wlg": _blk(w_in[:, 4096:5120]),
        "wga": _blk(w_in[:, 5120:6144]), "wgl": _blk(w_in[:, 6144:7168]),
        "wao": _blk(np.asarray(inp["w_attn_o"][0], f32)), "wlo": _blk(np.asarray(inp["w_lru_o"][0], f32)),
        "bg": np.ascontiguousarray(np.asarray(inp["b_gates"][0], f32).reshape(16, 128).T),
        "wout": np.ascontiguousarray(np.asarray(inp["w_out"][0], f32).reshape(8, 128, 1024).transpose(1, 0, 2)),
        "g2": np.asarray(inp["norm2_gain"][0][None, :], f32),
        "wr": np.ascontiguousarray(np.concatenate([inp["w_group_router"][0], inp["w_expert_router"][0]], 1).astype(f32).reshape(8, 128, 20).transpose(1, 0, 2)),
        "br": np.concatenate([inp["b_group_router"][0], inp["b_expert_router"][0]]).astype(f32)[None, :],
        "weg": np.ascontiguousarray(np.asarray(inp["w_expert_gate"][0], f32).reshape(16, 8, 128, 512).transpose(0, 2, 1, 3)),
        "weu": np.ascontiguousarray(np.asarray(inp["w_expert_up"][0], f32).reshape(16, 8, 128, 512).transpose(0, 2, 1, 3)),
        "wed": np.ascontiguousarray(np.asarray(inp["w_expert_down"][0], f32).reshape(16, 4, 128, 1024).transpose(0, 2, 1, 3)),
    }
    conv_w = np.asarray(inp["conv_w"][0], f32)
    conv_b = np.asarray(inp["conv_b"][0], f32)
    wa = np.asarray(inp["lru_wa"][0], f32); wi = np.asarray(inp["lru_wi"][0], f32)
    ba = np.asarray(inp["lru_ba"][0], f32); bi = np.asarray(inp["lru_bi"][0], f32)
    lam = np.asarray(inp["lru_lambda"][0], f32)
    per_half = []
    for half in range(2):
        dA, dB = (0, 1) if half == 0 else (1, 0)
        cp = np.zeros((128, 8, 12), f32)
        taps = [None, conv_w[0], conv_w[1], conv_w[2], conv_w[3]] if half == 0 else [conv_w[3], conv_w[2], conv_w[1], conv_w[0], None]
        for j, tp in enumerate(taps):
            if tp is not None:
                cp[:, :, j] = _pc(tp)
        cp[:, :, 5] = _pc(conv_b)
        cp[:, :, 6] = _pc(ba[dA]); cp[:, :, 7] = _pc(ba[dB]); cp[:, :, 8] = _pc(bi[dA]); cp[:, :, 9] = _pc(bi[dB])
        cp[:, :, 10] = _pc(lam[dA]); cp[:, :, 11] = _pc(lam[dB])
        wg = np.zeros((8, 128, 4, 128), f32)
        for n in range(8):
            wg[n, :, 0] = wa[dA, n]; wg[n, :, 1] = wi[dA, n]; wg[n, :, 2] = wa[dB, n]; wg[n, :, 3] = wi[dB, n]
        cs = cs_f if half == 0 else np.ascontiguousarray(cs_f[::-1])
        per_half.append({"cp": cp, "wg": wg, "cs": cs})
    maps = []
    for c in range(2 * B):
        b, half = c // 2, c % 2
        xb = np.asarray(inp["x"][b], f32)
        m = dict(shared)
        m.update(per_half[half])
        m["x"] = xb if half == 0 else np.ascontiguousarray(xb[::-1])
        maps.append(m)
    return maps


def assemble(results, B, T):
    TO = T // 2
    out = np.zeros((B, T, 1024), np.float32)
    for c in range(2 * B):
        b, half = c // 2, c % 2
        r = np.asarray(results[c]["out"], np.float32)
        if half == 0:
            out[b, :TO] = r
        else:
            out[b, TO:] = r[::-1]
    return out


def kernel(**inputs):
    T = inputs["x"].shape[1]
    B = inputs["x"].shape[0]
    key = (T,)
    if key not in _NC_CACHE:
        _NC_CACHE[key] = build(Cfg(T=T))
    nc = _NC_CACHE[key]
    maps = make_in_maps(inputs, T)
    res = run_bass_kernel_spmd(nc, maps, core_ids=list(range(2 * B)))
    return assemble(res.results, B, T)
```
